# Optimizing a Trainium2 kernel written in Bass

```python
import jax
import jax.numpy as jnp
from jax import lax
import numpy as np

D_MODEL = 4096
BATCH = 1
SEQ = 8192
DEPTH = 1

CHUNK = 64
NORM_EPS = 1e-6

M_HEADS = 4
M_QK_DIM = 256
M_V_DIM = 512
M_WIDTH = M_HEADS * M_V_DIM
CONV_WIDTH = 4
GATE_SOFTCAP = 15.0

A_HEADS = 16
A_NOPE_DIM = 128
A_ROPE_DIM = 64
A_V_DIM = 128
A_Q_RANK = 768
A_KV_RANK = 512
A_WIDTH = A_HEADS * A_V_DIM
ROPE_THETA = 10000.0
Q_BLOCK = 128

MIX_WIDTH = M_WIDTH + A_WIDTH

IN_SIZES = (2 * M_HEADS * M_QK_DIM, M_WIDTH, M_WIDTH, M_HEADS, M_HEADS, A_Q_RANK, A_KV_RANK, A_ROPE_DIM)
N_IN = sum(IN_SIZES)

N_EXPERTS = 64
EXPERT_FF = 512
SHARED_FF = 512
TOP_K = 8
N_GROUPS = 8
TOPK_GROUPS = 4
ROUTED_SCALE = 2.5
EXPERT_BLOCK = 128

kernel_name = 'hybrid_mlstm_mla_moe_block'


def rms_norm(x, g):
    xf = x.astype(jnp.float32)
    y = xf * lax.rsqrt(jnp.mean(xf * xf, axis=-1, keepdims=True) + NORM_EPS)
    return (y * g.astype(jnp.float32)).astype(x.dtype)


def softcap(z, cap):
    return cap * jnp.tanh(z / cap)


def rope_tables(seq_len):
    pos = jnp.arange(seq_len, dtype=jnp.float32)
    inv_freq = 1.0 / (ROPE_THETA ** (jnp.arange(0, A_ROPE_DIM, 2, dtype=jnp.float32) / A_ROPE_DIM))
    ang = pos[:, None] * inv_freq[None, :]
    return jnp.cos(ang), jnp.sin(ang)


def apply_rope(x, cos, sin):
    half = x.shape[-1] // 2
    x1 = x[..., :half].astype(jnp.float32)
    x2 = x[..., half:].astype(jnp.float32)
    return jnp.concatenate([x1 * cos - x2 * sin, x2 * cos + x1 * sin], axis=-1).astype(x.dtype)


def causal_depthwise_conv(u, w, b):
    out = lax.conv_general_dilated(u, w[:, None, :].astype(u.dtype), window_strides=(1,),
                                   padding=[(CONV_WIDTH - 1, 0)],
                                   dimension_numbers=('NWC', 'WIO', 'NWC'),
                                   feature_group_count=u.shape[-1])
    return out + b


def mlstm_chunkwise(q, k, v, i_pre, f_pre):
    b_sz, s_len, n_h, d_k = q.shape
    d_v = v.shape[-1]
    n_c = s_len // CHUNK

    def chunks(t):
        return t.astype(jnp.float32).reshape(b_sz, n_c, CHUNK, n_h, -1).transpose(1, 0, 3, 2, 4)

    qc = chunks(q) * (d_k ** -0.5)
    kc = chunks(k)
    vc = chunks(v)
    log_i = chunks(i_pre[..., None])[..., 0]
    log_f = chunks(jax.nn.log_sigmoid(f_pre)[..., None])[..., 0]
    causal = jnp.tril(jnp.ones((CHUNK, CHUNK), dtype=bool))

    def step(carry, inp):
        c_state, n_state, m_state = carry
        q_t, k_t, v_t, li, lf = inp
        b = jnp.cumsum(lf, axis=-1)
        d_mat = jnp.where(causal, b[..., :, None] - b[..., None, :] + li[..., None, :], -jnp.inf)
        inter = b + m_state[..., None]
        m_t = jnp.maximum(jnp.max(d_mat, axis=-1), inter)
        decay_in = jnp.exp(inter - m_t)
        s = jnp.einsum('bhtd,bhsd->bhts', q_t, k_t) * jnp.exp(d_mat - m_t[..., None])
        num = jnp.einsum('bhts,bhse->bhte', s, v_t) + decay_in[..., None] * jnp.einsum('bhtd,bhde->bhte', q_t, c_state)
        den = jnp.sum(s, axis=-1) + decay_in * jnp.einsum('bhtd,bhd->bht', q_t, n_state)
        h = num / jnp.maximum(jnp.abs(den), jnp.exp(-m_t))[..., None]
        b_last = b[..., -1]
        w_log = b_last[..., None] - b + li
        m_new = jnp.maximum(b_last + m_state, jnp.max(w_log, axis=-1))
        carry_decay = jnp.exp(b_last + m_state - m_new)
        w = jnp.exp(w_log - m_new[..., None])
        c_new = carry_decay[..., None, None] * c_state + jnp.einsum('bhs,bhsd,bhse->bhde', w, k_t, v_t)
        n_new = carry_decay[..., None] * n_state + jnp.einsum('bhs,bhsd->bhd', w, k_t)
        return (c_new, n_new, m_new), h

    init = (jnp.zeros((b_sz, n_h, d_k, d_v), jnp.float32),
            jnp.zeros((b_sz, n_h, d_k), jnp.float32),
            jnp.zeros((b_sz, n_h), jnp.float32))
    _, hc = lax.scan(step, init, (qc, kc, vc, log_i, log_f))
    return hc.transpose(1, 0, 3, 2, 4).reshape(b_sz, s_len, n_h, d_v)


def mla_attention(c_q, c_kv, k_rope, g_q_norm, w_uq, g_kv_norm, w_ukv, cos, sin, chunk_id):
    b_sz, s_len, _ = c_q.shape
    q = (rms_norm(c_q, g_q_norm) @ w_uq).reshape(b_sz, s_len, A_HEADS, A_NOPE_DIM + A_ROPE_DIM)
    q_nope = q[..., :A_NOPE_DIM]
    q_rope = apply_rope(q[..., A_NOPE_DIM:], cos[:, None, :], sin[:, None, :])
    kv = (rms_norm(c_kv, g_kv_norm) @ w_ukv).reshape(b_sz, s_len, A_HEADS, A_NOPE_DIM + A_V_DIM)
    k_nope = kv[..., :A_NOPE_DIM]
    v = kv[..., A_NOPE_DIM:]
    k_pe = apply_rope(k_rope, cos, sin)
    scale = (A_NOPE_DIM + A_ROPE_DIM) ** -0.5
    n_qb = s_len // Q_BLOCK

    def blocks(t):
        return t.reshape(b_sz, n_qb, Q_BLOCK, *t.shape[2:]).swapaxes(0, 1)

    def attend(args):
        qn, qr, q_cid = args
        s = jnp.einsum('bqhd,bkhd->bhqk', qn, k_nope) + jnp.einsum('bqhd,bkd->bhqk', qr, k_pe)
        s = s.astype(jnp.float32) * scale
        mask = chunk_id[None, :] <= q_cid[:, None]
        p = jax.nn.softmax(jnp.where(mask, s, -jnp.inf), axis=-1).astype(v.dtype)
        return jnp.einsum('bhqk,bkhd->bqhd', p, v)

    out = lax.map(attend, (blocks(q_nope), blocks(q_rope), chunk_id.reshape(n_qb, Q_BLOCK)))
    return out.swapaxes(0, 1).reshape(b_sz, s_len, A_WIDTH)


def token_mixer(h, w_in, conv_w, conv_b, b_igate, b_fgate, g_mlstm_out, g_q_norm, w_uq,
                g_kv_norm, w_ukv, w_out, cos, sin, chunk_id):
    b_sz, s_len, _ = h.shape
    proj = h @ w_in
    split_points = np.cumsum(IN_SIZES)[:-1].tolist()
    qk, v_m, o_m, i_m, f_m, c_q, c_kv, k_rope = jnp.split(proj, split_points, axis=-1)
    qk = jax.nn.silu(causal_depthwise_conv(qk, conv_w, conv_b))
    q_m, k_m = jnp.split(qk, 2, axis=-1)
    shp = (b_sz, s_len, M_HEADS)
    i_pre = softcap(i_m.astype(jnp.float32) + b_igate.astype(jnp.float32), GATE_SOFTCAP)
    f_pre = softcap(f_m.astype(jnp.float32) + b_fgate.astype(jnp.float32), GATE_SOFTCAP)
    h_m = mlstm_chunkwise(q_m.reshape(*shp, M_QK_DIM), k_m.reshape(*shp, M_QK_DIM),
                          v_m.reshape(*shp, M_V_DIM), i_pre, f_pre)
    h_m = rms_norm(h_m, g_mlstm_out.reshape(M_HEADS, M_V_DIM)) * jax.nn.sigmoid(o_m.astype(jnp.float32)).reshape(*shp, M_V_DIM)
    h_m = h_m.reshape(b_sz, s_len, M_WIDTH).astype(h.dtype)
    h_a = mla_attention(c_q, c_kv, k_rope, g_q_norm, w_uq, g_kv_norm, w_ukv, cos, sin, chunk_id)
    return jnp.concatenate([h_m, h_a], axis=-1) @ w_out


def swiglu(h, w_g, w_u, w_d):
    return (jax.nn.silu(h @ w_g) * (h @ w_u)) @ w_d


def route(h, w_router, b_router):
    n_tok = h.shape[0]
    scores = jax.nn.sigmoid(h.astype(jnp.float32) @ w_router.astype(jnp.float32))
    biased = scores + b_router.astype(jnp.float32)
    grp = biased.reshape(n_tok, N_GROUPS, N_EXPERTS // N_GROUPS)
    grp_score = jnp.sum(lax.top_k(grp, 2)[0], axis=-1)
    _, top_grp = lax.top_k(grp_score, TOPK_GROUPS)
    grp_mask = jnp.any(top_grp[..., None] == jnp.arange(N_GROUPS)[None, None, :], axis=1)
    expert_mask = jnp.repeat(grp_mask, N_EXPERTS // N_GROUPS, axis=1)
    _, idx = lax.top_k(jnp.where(expert_mask, biased, -jnp.inf), TOP_K)
    wts = jnp.take_along_axis(scores, idx, axis=-1)
    wts = wts / jnp.sum(wts, axis=-1, keepdims=True) * ROUTED_SCALE
    return idx, wts


def routed_experts(h, idx, wts, w_gate, w_up, w_down):
    n_tok, d = h.shape
    n_assign = n_tok * TOP_K
    n_slots = -(-(n_assign + N_EXPERTS * (EXPERT_BLOCK - 1)) // EXPERT_BLOCK) * EXPERT_BLOCK
    n_blocks = n_slots // EXPERT_BLOCK
    flat_e = idx.reshape(-1).astype(jnp.int32)
    flat_tok = jnp.repeat(jnp.arange(n_tok, dtype=jnp.int32), TOP_K)
    flat_w = wts.reshape(-1)
    order = jnp.argsort(flat_e)
    sorted_e = flat_e[order]
    counts = jnp.bincount(flat_e, length=N_EXPERTS).astype(jnp.int32)
    padded = (counts + EXPERT_BLOCK - 1) // EXPERT_BLOCK * EXPERT_BLOCK
    ends_pad = jnp.cumsum(padded)
    starts_pad = ends_pad - padded
    starts = jnp.cumsum(counts) - counts
    dest = starts_pad[sorted_e] + jnp.arange(n_assign, dtype=jnp.int32) - starts[sorted_e]
    slot_tok = jnp.zeros((n_slots,), jnp.int32).at[dest].set(flat_tok[order])
    slot_w = jnp.zeros((n_slots,), jnp.float32).at[dest].set(flat_w[order])
    block_start = jnp.arange(n_blocks, dtype=jnp.int32) * EXPERT_BLOCK
    block_e = jnp.minimum(jnp.searchsorted(ends_pad, block_start, side='right'), N_EXPERTS - 1)

    def expert_block(args):
        tok, e = args
        xb = h[tok]
        return (jax.nn.silu(xb @ w_gate[e]) * (xb @ w_up[e])) @ w_down[e]

    yb = lax.map(expert_block, (slot_tok.reshape(n_blocks, EXPERT_BLOCK), block_e))
    y = yb.reshape(n_slots, d) * slot_w[:, None].astype(yb.dtype)
    return jnp.zeros((n_tok, d), h.dtype).at[slot_tok].add(y.astype(h.dtype))


def moe_ffn(h, w_router, b_router, w_gate, w_up, w_down, w_shared_gate, w_shared_up, w_shared_down):
    idx, wts = route(h, w_router, b_router)
    return routed_experts(h, idx, wts, w_gate, w_up, w_down) + swiglu(h, w_shared_gate, w_shared_up, w_shared_down)


def setup_inputs(seed: int = 0) -> dict:
    key = jax.random.key(seed)
    ks = jax.random.split(key, 32)
    f32 = jnp.float32
    L = DEPTH

    def nrm(k, shape, scale):
        return jax.random.normal(k, shape, f32) * scale

    def gain(k, n):
        return 1.0 + 0.02 * jax.random.normal(k, (L, n), f32)

    return {
        'x': nrm(ks[0], (BATCH, SEQ, D_MODEL), 1.0),
        'c': nrm(ks[1], (BATCH, D_MODEL), 1.0),
        'w_ada': nrm(ks[2], (L, D_MODEL, 6 * D_MODEL), 0.5 * D_MODEL ** -0.5),
        'b_ada': nrm(ks[3], (L, 6 * D_MODEL), 0.02),
        'g_pre_mix': gain(ks[4], D_MODEL),
        'g_post_mix': gain(ks[5], D_MODEL),
        'w_in': nrm(ks[6], (L, D_MODEL, N_IN), D_MODEL ** -0.5),
        'conv_w': nrm(ks[7], (L, CONV_WIDTH, 2 * M_HEADS * M_QK_DIM), CONV_WIDTH ** -0.5),
        'conv_b': nrm(ks[8], (L, 2 * M_HEADS * M_QK_DIM), 0.02),
        'b_igate': nrm(ks[9], (L, M_HEADS), 0.5),
        'b_fgate': 3.0 + nrm(ks[10], (L, M_HEADS), 0.5),
        'g_mlstm_out': gain(ks[11], M_WIDTH),
        'g_q_norm': gain(ks[12], A_Q_RANK),
        'w_uq': nrm(ks[13], (L, A_Q_RANK, A_HEADS * (A_NOPE_DIM + A_ROPE_DIM)), A_Q_RANK ** -0.5),
        'g_kv_norm': gain(ks[14], A_KV_RANK),
        'w_ukv': nrm(ks[15], (L, A_KV_RANK, A_HEADS * (A_NOPE_DIM + A_V_DIM)), A_KV_RANK ** -0.5),
        'w_out': nrm(ks[16], (L, MIX_WIDTH, D_MODEL), MIX_WIDTH ** -0.5),
        'g_pre_ffn': gain(ks[17], D_MODEL),
        'g_post_ffn': gain(ks[18], D_MODEL),
        'w_router': nrm(ks[19], (L, D_MODEL, N_EXPERTS), D_MODEL ** -0.5),
        'b_router': nrm(ks[20], (L, N_EXPERTS), 0.01),
        'w_gate': nrm(ks[21], (L, N_EXPERTS, D_MODEL, EXPERT_FF), D_MODEL ** -0.5),
        'w_up': nrm(ks[22], (L, N_EXPERTS, D_MODEL, EXPERT_FF), D_MODEL ** -0.5),
        'w_down': nrm(ks[23], (L, N_EXPERTS, EXPERT_FF, D_MODEL), EXPERT_FF ** -0.5),
        'w_shared_gate': nrm(ks[24], (L, D_MODEL, SHARED_FF), D_MODEL ** -0.5),
        'w_shared_up': nrm(ks[25], (L, D_MODEL, SHARED_FF), D_MODEL ** -0.5),
        'w_shared_down': nrm(ks[26], (L, SHARED_FF, D_MODEL), SHARED_FF ** -0.5),
    }


def reference(x, c, w_ada, b_ada, g_pre_mix, g_post_mix, w_in, conv_w, conv_b, b_igate, b_fgate,
              g_mlstm_out, g_q_norm, w_uq, g_kv_norm, w_ukv, w_out, g_pre_ffn, g_post_ffn,
              w_router, b_router, w_gate, w_up, w_down, w_shared_gate, w_shared_up, w_shared_down):
    b_sz, s_len, d = x.shape
    cos, sin = rope_tables(s_len)
    chunk_id = jnp.arange(s_len, dtype=jnp.int32) // CHUNK
    c_act = jax.nn.silu(c)
    for l in range(DEPTH):
        mod = c_act @ w_ada[l] + b_ada[l]
        sh1, sc1, gt1, sh2, sc2, gt2 = [m[:, None, :] for m in jnp.split(mod, 6, axis=-1)]
        h = rms_norm(x, g_pre_mix[l]) * (1.0 + sc1) + sh1
        y = token_mixer(h, w_in[l], conv_w[l], conv_b[l], b_igate[l], b_fgate[l], g_mlstm_out[l],
                        g_q_norm[l], w_uq[l], g_kv_norm[l], w_ukv[l], w_out[l], cos, sin, chunk_id)
        x = x + gt1 * rms_norm(y, g_post_mix[l])
        h = rms_norm(x, g_pre_ffn[l]) * (1.0 + sc2) + sh2
        y = moe_ffn(h.reshape(b_sz * s_len, d), w_router[l], b_router[l], w_gate[l], w_up[l], w_down[l],
                    w_shared_gate[l], w_shared_up[l], w_shared_down[l]).reshape(b_sz, s_len, d)
        x = x + gt2 * rms_norm(y, g_post_ffn[l])
    return x
```

```python
import types
import numpy as np
import concourse.bass as bass
import concourse.mybir as mybir
from concourse.bass_utils import run_bass_kernel_spmd

F32 = mybir.dt.float32
BF16 = mybir.dt.bfloat16
U8 = mybir.dt.uint8
AF = mybir.ActivationFunctionType
ALU = mybir.AluOpType
AX = mybir.AxisListType

ENG = ["sync", "scalar", "vector", "gpsimd", "tensor"]
NCORES = 8
D = 4096
SEQ = 8192
TPC = SEQ // NCORES
EPS = 1e-6


class Sched:
    def __init__(self, nc):
        self.nc = nc
        self.q = {e: [] for e in ENG}
        self.cnt = {e: 0 for e in ENG}
        self.seen = {e: {} for e in ENG}
        self.sems = {e: nc.alloc_semaphore("s_" + e) for e in ENG}
        self.dsem = {}
        self.dcnt = {}
        self.debug = False
        self.snaps = []

    def _waits(self, eng, deps):
        w = []
        for d in deps:
            if d is None:
                continue
            key, val = d
            if self.seen[eng].get(key, 0) >= val:
                continue
            self.seen[eng][key] = val
            w.append((key, val))
        return w

    def _snap(self, fn):
        if fn is None or fn.__closure__ is None:
            return None
        out = []
        for c in fn.__closure__:
            try:
                out.append(c.cell_contents)
            except ValueError:
                out.append(None)
        return out

    def _check(self, fn, snap):
        if snap is None:
            return
        for name, c, old in zip(fn.__code__.co_freevars, fn.__closure__, snap):
            assert c.cell_contents is old, f"late-bound closure variable '{name}' in lambda at line {fn.__code__.co_firstlineno}"

    @staticmethod
    def _freeze(fn):
        if fn is None or fn.__closure__ is None:
            return fn
        cells = []
        for c in fn.__closure__:
            try:
                cells.append(types.CellType(c.cell_contents))
            except ValueError:
                cells.append(c)
        return types.FunctionType(fn.__code__, fn.__globals__, fn.__name__, fn.__defaults__, tuple(cells))

    def op(self, eng, fn, deps=(), signal=True):
        fn = self._freeze(fn)
        if self.debug:
            self.snaps.append((fn, self._snap(fn)))
        w = self._waits(eng, deps)
        tok = None
        if signal:
            self.cnt[eng] += 1
            tok = (eng, self.cnt[eng])
        self.q[eng].append((fn, w, ("e", eng) if signal else None))
        return tok

    def dma(self, eng, slot, out, in_, deps=(), **kw):
        if slot not in self.dsem:
            self.dsem[slot] = self.nc.alloc_semaphore("d_" + slot)
            self.dcnt[slot] = 0
        w = self._waits(eng, deps)
        self.dcnt[slot] += 16
        self.q[eng].append((lambda e: e.dma_start(out=out, in_=in_, **kw), w, ("d", slot)))
        return ("d:" + slot, self.dcnt[slot])

    def fence(self):
        toks = [(e, self.cnt[e]) for e in ENG if self.cnt[e] > 0]
        toks += [("d:" + s, v) for s, v in self.dcnt.items()]
        for e in ENG:
            w = self._waits(e, toks)
            if w:
                self.q[e].append((None, w, None))

    def _sem(self, key):
        if key.startswith("d:"):
            return self.dsem[key[2:]]
        return self.sems[key]

    def emit(self, final_deps=()):
        nc = self.nc
        for fn, snap in self.snaps:
            self._check(fn, snap)
        w = self._waits("sync", final_deps)
        self.q["sync"].append((None, w, None))
        with nc.Block() as block:
            def run(ename):
                def body(e):
                    for fn, waits, inc in self.q[ename]:
                        for key, val in waits:
                            e.wait_ge(self._sem(key), val)
                        if fn is None:
                            continue
                        ins = fn(e)
                        if inc is not None:
                            if inc[0] == "e":
                                ins.then_inc(self.sems[inc[1]], 1)
                            else:
                                ins.then_inc(self.dsem[inc[1]], 16)
                return body
            block.sync(run("sync"))
            block.scalar(run("scalar"))
            block.vector(run("vector"))
            block.gpsimd(run("gpsimd"))
            block.tensor(run("tensor"))


class Arena:
    def __init__(self, nc, nbytes, name="arena"):
        self.t8 = nc.alloc_sbuf_tensor(name, [128, nbytes], U8)
        self.t16 = self.t8.bitcast(BF16)
        self.t32 = self.t8.bitcast(F32)
        self.n = nbytes
        self.off = 0

    def mark(self):
        return self.off

    def release(self, m):
        self.off = m

    def alloc(self, shape, dt):
        esz = 4 if dt == F32 else 2
        free = 1
        for s in shape[1:]:
            free *= s
        nb = (free * esz + 63) // 64 * 64
        assert self.off + nb <= self.n, f"arena overflow: need {self.off + nb} > {self.n}"
        base = self.t32 if dt == F32 else self.t16
        o = self.off // esz
        self.off += nb
        ap = base[0:shape[0], o:o + free]
        if len(shape) == 3:
            ap = ap.rearrange("p (a b) -> p a b", a=shape[1])
        elif len(shape) == 4:
            ap = ap.rearrange("p (a b c) -> p a b c", a=shape[1], b=shape[2])
        return ap


class Rot:
    def __init__(self, bufs):
        self.bufs = list(bufs)
        self.free = [[] for _ in bufs]
        self.i = 0

    def get(self):
        k = self.i % len(self.bufs)
        self.i += 1
        deps = self.free[k]
        self.free[k] = []
        return k, self.bufs[k], deps

    def done(self, k, tok):
        self.free[k].append(tok)


def psum_banks(nc, n, name="pb"):
    return [nc.alloc_psum_tensor(f"{name}{i}", [128, 512], F32) for i in range(n)]


def load_consts(nc, S, A):
    cst = {}
    idn = nc.inline_tensor(np.eye(128, dtype=np.float32), "c_ident")
    cst["idf"] = A.alloc([128, 128], F32)
    cst["idb"] = A.alloc([128, 128], BF16)
    cst["onesf"] = A.alloc([128, 128], F32)
    cst["onesb"] = A.alloc([128, 128], BF16)
    t1 = S.dma("sync", "cst", cst["idf"], idn.ap())
    t2 = S.dma("gpsimd", "cst2", cst["idb"], idn.ap())
    t3 = S.op("vector", lambda e: e.memset(cst["onesf"], 1.0))
    t4 = S.op("vector", lambda e: e.memset(cst["onesb"], 1.0))
    cst["tok"] = [t1, t2, t3, t4]
    return cst


def adaln_rows(nc, S, A, cst, pb, c_d, wada_d, bada_d, nseg, row_hook):
    m = A.mark()
    c32 = A.alloc([32, 128], F32)
    cT = A.alloc([128, 32], F32)
    brow = A.alloc([1, D], F32)
    row = A.alloc([1, D], F32)
    KQ = 4
    wbuf = [A.alloc([128, KQ, 512], F32) for _ in range(3)]
    wrot = Rot(wbuf)
    t = S.dma("sync", "ada_c", c32, c_d.rearrange("o (a b) -> (o a) b", b=128))
    t = S.op("scalar", lambda e: e.activation(c32, c32, AF.Silu), deps=[t])
    pt = pb[0]
    t = S.op("tensor", lambda e: e.matmul(pt[:, 0:32], c32, cst["idf"][0:32, 0:32], start=True, stop=True),
             deps=[t] + cst["tok"])
    t_cT = S.op("vector", lambda e: e.tensor_copy(cT, pt[:, 0:32]), deps=[t])
    wv = wada_d.rearrange("(kc p) n -> p kc n", p=128)
    prow = Rot([pb[1], pb[2]])
    last_row_readers = []
    for seg in range(nseg):
        tb = S.dma("sync", "ada_b", brow, bada_d[0:1, seg * D:(seg + 1) * D], deps=last_row_readers)
        evs = []
        for cc in range(8):
            col0 = seg * D + cc * 512
            kp, ps, pdeps = prow.get()
            mmt = None
            for kq in range(32 // KQ):
                kb, wb_, wdeps = wrot.get()
                tw = S.dma("sync", f"ada_w{kb}", wb_, wv[:, kq * KQ:(kq + 1) * KQ, col0:col0 + 512], deps=wdeps)
                for k4 in range(KQ):
                    kc = kq * KQ + k4
                    mmt = S.op("tensor",
                               lambda e, ps=ps, kc=kc, wb_=wb_, k4=k4: e.matmul(
                                   ps[0:1, :], cT[:, kc:kc + 1], wb_[:, k4, :], start=(kc == 0), stop=(kc == 31)),
                               deps=[tw, t_cT] + (pdeps if kc == 0 else []), signal=(k4 == KQ - 1))
                wrot.done(kb, mmt)
            ev = S.op("vector", lambda e, ps=ps, cc=cc: e.tensor_tensor(
                row[0:1, cc * 512:(cc + 1) * 512], ps[0:1, :], brow[0:1, cc * 512:(cc + 1) * 512], ALU.add),
                deps=[mmt, tb] + last_row_readers)
            prow.done(kp, ev)
            evs.append(ev)
        last_row_readers = row_hook(seg, row, evs[-1])
    A.release(m)
    return last_row_readers


def row_to_cols(S, cst, pbank, row, out_cols, deps):
    t = None
    for c in range(32):
        t = S.op("tensor", lambda e, c=c: e.matmul(pbank[:, c:c + 1], row[0:1, c * 128:(c + 1) * 128],
                                                   cst["onesf"][0:1, 0:1], start=True, stop=True),
                 deps=deps if c == 0 else [], signal=(c == 31))
    return S.op("vector", lambda e: e.tensor_copy(out_cols, pbank[:, 0:32]), deps=[t])


def row_to_bcast(S, cst, prot, row, out_b, deps):
    toks = []
    for ch in range(8):
        kp, ps, pdeps = prot.get()
        t = S.op("tensor", lambda e, ps=ps, ch=ch: e.matmul(ps[:, :], cst["onesf"][0:1, 0:128],
                                                             row[0:1, ch * 512:(ch + 1) * 512], start=True, stop=True),
                 deps=deps + pdeps)
        t2 = S.op("vector", lambda e, ps=ps, ch=ch: e.tensor_copy(out_b[:, ch * 512:(ch + 1) * 512], ps[:, :]),
                  deps=[t])
        prot.done(kp, t2)
        toks.append(t2)
    return toks


def prenorm(S, A, cst, pb, x_src, ntiles, acol, bcol, ab_tok, hT, f32_hook=None, x_tiles=None):
    m = A.mark()
    xb = [A.alloc([128, D], F32) for _ in range(2)]
    junk = A.alloc([128, D], BF16)
    ss = A.alloc([128, 16], F32)
    stg = [A.alloc([128, 128], F32) for _ in range(4)]
    xrot = Rot(xb)
    prot = Rot(pb[0:4])
    srot = Rot(stg)
    junk_free = [S.op("vector", lambda e: e.memset(ss, 0.0))]
    last = []
    for tt in range(ntiles):
        kx, xt, xdeps = xrot.get()
        tl = S.dma("sync", f"pn_x{kx}", xt, x_src(tt), deps=xdeps)
        tsq = S.op("scalar", lambda e, xt=xt, tt=tt: e.activation(junk, xt, AF.Square, accum_out=ss[:, tt:tt + 1]),
                   deps=[tl] + junk_free)
        junk_free = [tsq]
        t1 = S.op("vector", lambda e, tt=tt: e.tensor_scalar(ss[:, tt:tt + 1], ss[:, tt:tt + 1], 1.0 / D, EPS,
                                                              ALU.mult, ALU.add), deps=[tsq])
        t2a = S.op("scalar", lambda e, tt=tt: e.activation(ss[:, tt:tt + 1], ss[:, tt:tt + 1], AF.Sqrt), deps=[t1])
        t2 = S.op("vector", lambda e, tt=tt: e.reciprocal(ss[:, tt:tt + 1], ss[:, tt:tt + 1]), deps=[t2a])
        t3 = S.op("vector", lambda e, xt=xt, tt=tt: e.tensor_scalar(xt, xt, ss[:, tt:tt + 1], None, ALU.mult),
                  deps=[t2])
        evs = []
        for c in range(32):
            kp, ps, pdeps = prot.get()
            ttr = S.op("tensor", lambda e, ps=ps, xt=xt, c=c: e.transpose(ps[:, 0:128], xt[:, c * 128:(c + 1) * 128],
                                                                          cst["idf"]),
                       deps=[t3] + pdeps + cst["tok"])
            if f32_hook is None:
                ev = S.op("scalar", lambda e, ps=ps, c=c, tt=tt: e.activation(
                    hT[:, c, tt * 128:(tt + 1) * 128], ps[:, 0:128], AF.Identity,
                    bias=bcol[:, c:c + 1], scale=acol[:, c:c + 1]), deps=[ttr] + ab_tok)
                prot.done(kp, ev)
            else:
                ks, sg, sdeps = srot.get()
                ev0 = S.op("scalar", lambda e, ps=ps, c=c, sg=sg: e.activation(
                    sg, ps[:, 0:128], AF.Identity, bias=bcol[:, c:c + 1], scale=acol[:, c:c + 1]),
                    deps=[ttr] + ab_tok + sdeps)
                prot.done(kp, ev0)
                ev = S.op("gpsimd", lambda e, c=c, tt=tt, sg=sg: e.tensor_copy(hT[:, c, tt * 128:(tt + 1) * 128], sg),
                          deps=[ev0])
                srot.done(ks, ev)
                for tk in f32_hook(tt, c, sg, ev0):
                    srot.done(ks, tk)
            evs.append(ev)
        xrot.done(kx, evs[-1])
        last = [evs[-1]]
    A.release(m)
    return last


NF_A = 2048 + 768 + 512 + 64 + 64
NT_A = 2048 + 2048 + 8


def win_col_perm():
    sizes = [2048, 2048, 2048, 4, 4, 768, 512, 64]
    offs = np.cumsum([0] + sizes)
    qk, v, o, ig, fg, cq, ckv, kr = [np.arange(offs[i], offs[i + 1]) for i in range(8)]
    krs = np.concatenate([kr[32:], kr[:32]])
    return np.concatenate([qk, cq, ckv, kr, krs, v, o, ig, fg])


def build_phase_a():
    nc = bass.Bass("TRN2", target_bir_lowering=False)
    x_d = nc.dram_tensor("x", [TPC, D], F32, kind="ExternalInput").ap()
    c_d = nc.dram_tensor("c", [1, D], F32, kind="ExternalInput").ap()
    wada_d = nc.dram_tensor("wada", [D, 2 * D], F32, kind="ExternalInput").ap()
    bada_d = nc.dram_tensor("bada", [1, 2 * D], F32, kind="ExternalInput").ap()
    g_d = nc.dram_tensor("gpre", [1, D], F32, kind="ExternalInput").ap()
    win_d = nc.dram_tensor("win", [D, NF_A + NT_A], F32, kind="ExternalInput").ap()
    featT_d = nc.dram_tensor("featT", [NF_A, TPC], F32, kind="ExternalOutput").ap()
    tok_d = nc.dram_tensor("tokm", [TPC, NT_A], F32, kind="ExternalOutput").ap()
    S = Sched(nc)
    A = Arena(nc, 196 * 1024)
    pb = psum_banks(nc, 8)
    cst = load_consts(nc, S, A)
    acol = A.alloc([128, 32], F32)
    bcol = A.alloc([128, 32], F32)
    hT = A.alloc([128, 32, TPC], BF16)
    grow = A.alloc([1, D], F32)
    tg = S.dma("sync", "a_g", grow, g_d)
    ab_tok = []

    def hook(seg, row, tok):
        if seg == 0:
            t = row_to_cols(S, cst, pb[3], row, bcol, [tok])
        else:
            t0 = S.op("vector", lambda e: e.scalar_tensor_tensor(row, row, 1.0, grow, ALU.add, ALU.mult),
                      deps=[tok, tg])
            t = row_to_cols(S, cst, pb[3], row, acol, [t0])
        ab_tok.append(t)
        return [t]

    adaln_rows(nc, S, A, cst, pb, c_d, wada_d, bada_d, 2, hook)
    S.fence()
    prenorm(S, A, cst, pb, lambda tt: x_d[tt * 128:(tt + 1) * 128, :], TPC // 128, acol, bcol, ab_tok, hT)
    S.fence()
    outs = in_proj(S, A, pb, hT, win_d, featT_d, tok_d)
    S.emit(outs)
    return nc


def in_proj(S, A, pb, hT, win_d, featT_d, tok_d):
    m = A.mark()
    GW = 256
    wbuf = [A.alloc([128, 32, GW], BF16) for _ in range(3)]
    wrot = Rot(wbuf)
    stg = [A.alloc([128, 1024], F32) for _ in range(3)]
    srot = Rot(stg)
    prot = Rot(pb)
    wv = win_d.rearrange("(kc p) n -> p kc n", p=128)
    outs = []
    groups = [("f", c0, min(GW, NF_A - c0)) for c0 in range(0, NF_A, GW)]
    groups += [("t", c0, min(GW, NT_A - c0)) for c0 in range(0, NT_A, GW)]
    nev = 0
    for kind, c0, ncol in groups:
        gc0 = c0 if kind == "f" else NF_A + c0
        kw, wb_, wdeps = wrot.get()
        tw = S.dma("gpsimd", f"ip_w{kw}", wb_[:, :, 0:ncol], wv[:, :, gc0:gc0 + ncol], deps=wdeps)
        lastmm = None
        if kind == "f":
            for mc in range(0, ncol, 128):
                mw = min(128, ncol - mc)
                ks, sg, sdeps = srot.get()
                evs = []
                for th in range(TPC // 512):
                    kp, ps, pdeps = prot.get()
                    for kc in range(32):
                        lastmm = S.op("tensor", lambda e, ps=ps, kc=kc, wb_=wb_, mc=mc, mw=mw, th=th: e.matmul(
                            ps[0:mw, :], wb_[:, kc, mc:mc + mw], hT[:, kc, th * 512:(th + 1) * 512],
                            start=(kc == 0), stop=(kc == 31)), deps=[tw] + (pdeps if kc == 0 else []),
                            signal=(kc == 31))
                    eng = "vector" if nev % 2 == 0 else "scalar"
                    nev += 1
                    if eng == "vector":
                        ev = S.op(eng, lambda e, ps=ps, sg=sg, mw=mw, th=th: e.tensor_copy(
                            sg[0:mw, th * 512:(th + 1) * 512], ps[0:mw, :]), deps=[lastmm] + sdeps)
                    else:
                        ev = S.op(eng, lambda e, ps=ps, sg=sg, mw=mw, th=th: e.activation(
                            sg[0:mw, th * 512:(th + 1) * 512], ps[0:mw, :], AF.Copy), deps=[lastmm] + sdeps)
                    prot.done(kp, ev)
                    evs.append(ev)
                to = S.dma("sync", f"ip_o{ks}", featT_d[c0 + mc:c0 + mc + mw, :], sg[0:mw, :], deps=evs)
                srot.done(ks, to)
                outs.append(to)
        else:
            for tq in range(TPC // 512):
                ks, sg, sdeps = srot.get()
                sgv = sg.rearrange("p (a b) -> p a b", a=4)
                evs = []
                for t4 in range(4):
                    tt = tq * 4 + t4
                    kp, ps, pdeps = prot.get()
                    for kc in range(32):
                        lastmm = S.op("tensor", lambda e, ps=ps, kc=kc, wb_=wb_, tt=tt, ncol=ncol: e.matmul(
                            ps[:, 0:ncol], hT[:, kc, tt * 128:(tt + 1) * 128], wb_[:, kc, 0:ncol],
                            start=(kc == 0), stop=(kc == 31)), deps=[tw] + (pdeps if kc == 0 else []),
                            signal=(kc == 31))
                    eng = "vector" if nev % 2 == 0 else "scalar"
                    nev += 1
                    if eng == "vector":
                        ev = S.op(eng, lambda e, ps=ps, sgv=sgv, t4=t4, ncol=ncol: e.tensor_copy(
                            sgv[:, t4, 0:ncol], ps[:, 0:ncol]), deps=[lastmm] + sdeps)
                    else:
                        ev = S.op(eng, lambda e, ps=ps, sgv=sgv, t4=t4, ncol=ncol: e.activation(
                            sgv[:, t4, 0:ncol], ps[:, 0:ncol], AF.Copy), deps=[lastmm] + sdeps)
                    prot.done(kp, ev)
                    evs.append(ev)
                dst = tok_d[tq * 512:(tq + 1) * 512, c0:c0 + ncol].rearrange("(a p) n -> p a n", p=128)
                to = S.dma("sync", f"ip_o{ks}", dst, sgv[:, :, 0:ncol], deps=evs)
                srot.done(ks, to)
                outs.append(to)
        wrot.done(kw, lastmm)
    A.release(m)
    return outs


NE = 65


def row_to_bcast_n(S, cst, prot, row, out_b, ncols, deps):
    toks = []
    for ch in range(0, ncols, 512):
        w = min(512, ncols - ch)
        kp, ps, pdeps = prot.get()
        t = S.op("tensor", lambda e, ps=ps, ch=ch, w=w: e.matmul(ps[:, 0:w], cst["onesf"][0:1, 0:128],
                                                                  row[0:1, ch:ch + w], start=True, stop=True),
                 deps=deps + pdeps)
        t2 = S.op("vector", lambda e, ps=ps, ch=ch, w=w: e.tensor_copy(out_b[:, ch:ch + w], ps[:, 0:w]), deps=[t])
        prot.done(kp, t2)
        toks.append(t2)
    return toks


def build_phase_c():
    nc = bass.Bass("TRN2", target_bir_lowering=False)
    dt = lambda n, s, k="ExternalInput", d=F32: nc.dram_tensor(n, s, d, kind=k).ap()
    x_d = dt("x", [TPC, D]); c_d = dt("c", [1, D])
    wada_d = dt("wada", [D, 4 * D]); bada_d = dt("bada", [1, 4 * D])
    gpm_d = dt("gpostmix", [1, D]); gpf_d = dt("gpreffn", [1, D]); gpo_d = dt("gpostffn", [1, D])
    hm_d = dt("hm", [TPC, 2048]); og_d = dt("og", [TPC, 2048]); gmo_d = dt("gmo", [1, 2048])
    haT_d = dt("haT", [2048, TPC]); wout_d = dt("wout", [D, D])
    wr_d = dt("wr", [D, 64]); br_d = dt("br", [1, 64])
    wg_d = dt("wg", [NE, D, 512]); wu_d = dt("wu", [NE, D, 512]); wd_d = dt("wd", [NE, 512, D])
    out_d = dt("out", [TPC, D], "ExternalOutput")
    x1_d = dt("x1s", [TPC, D], "Internal")
    act_d = dt("acts", [NE * 512, TPC], "Internal", BF16)
    S = Sched(nc)
    A = Arena(nc, 196 * 1024)
    pb = psum_banks(nc, 8)
    cst = load_consts(nc, S, A)
    acol = A.alloc([128, 32], F32); bcol = A.alloc([128, 32], F32)
    B2 = A.alloc([128, D], F32)
    m_mix = A.mark()
    B1 = A.alloc([128, D], F32)
    gmo_b = A.alloc([128, 2048], F32)
    m_rows = A.mark()
    grow = [A.alloc([1, D], F32) for _ in range(3)]
    tg = [S.dma("sync", f"c_g{i}", grow[i], g) for i, g in enumerate([gpm_d, gpf_d, gpo_d])]
    gmrow = A.alloc([1, 2048], F32)
    tgm = S.dma("sync", "c_gm", gmrow, gmo_d)
    prot_s = Rot(pb[4:8])
    ab_tok = []
    mod_tok = []
    mod_tok += row_to_bcast_n(S, cst, prot_s, gmrow, gmo_b, 2048, [tgm] + cst["tok"])

    def hook(seg, row, tok):
        if seg == 0:
            t0 = S.op("vector", lambda e: e.tensor_tensor(row, row, grow[0], ALU.mult), deps=[tok, tg[0]])
            ts = row_to_bcast_n(S, cst, prot_s, row, B1, D, [t0])
            mod_tok.extend(ts)
            return ts
        if seg == 1:
            t = row_to_cols(S, cst, pb[3], row, bcol, [tok])
        elif seg == 2:
            t0 = S.op("vector", lambda e: e.scalar_tensor_tensor(row, row, 1.0, grow[1], ALU.add, ALU.mult),
                      deps=[tok, tg[1]])
            t = row_to_cols(S, cst, pb[3], row, acol, [t0])
        else:
            t0 = S.op("vector", lambda e: e.tensor_tensor(row, row, grow[2], ALU.mult), deps=[tok, tg[2]])
            ts = row_to_bcast_n(S, cst, prot_s, row, B2, D, [t0])
            mod_tok.extend(ts)
            return ts
        ab_tok.append(t)
        return [t]

    adaln_rows(nc, S, A, cst, pb, c_d, wada_d, bada_d, 4, hook)
    S.fence()
    A.release(m_rows)
    mixer_out(S, A, cst, pb, x_d, hm_d, og_d, gmo_b, haT_d, wout_d, B1, x1_d)
    S.fence()
    A.release(m_mix)
    h2T = A.alloc([128, 32, TPC], BF16)
    GT = A.alloc([64, TPC], F32)
    router_norm(S, A, cst, pb, x1_d, acol, bcol, ab_tok, h2T, wr_d, br_d, GT)
    S.fence()
    moe_m1(S, A, cst, pb, h2T, GT, wg_d, wu_d, act_d)
    S.fence()
    A.release(m_mix)
    outs = moe_m2_final(S, A, cst, pb, act_d, wd_d, x1_d, B2, out_d)
    S.emit(outs)
    return nc


def rstd_from_ss(S, ss_ap, n, deps):
    t1 = S.op("vector", lambda e: e.tensor_scalar(ss_ap, ss_ap, 1.0 / n, EPS, ALU.mult, ALU.add), deps=deps)
    t2 = S.op("scalar", lambda e: e.activation(ss_ap, ss_ap, AF.Sqrt), deps=[t1])
    return S.op("vector", lambda e: e.reciprocal(ss_ap, ss_ap), deps=[t2])


def mixer_out(S, A, cst, pb, x_d, hm_d, og_d, gmo_b, haT_d, wout_d, B1, x1_d):
    m = A.mark()
    HT = 256
    NT4 = HT // 128
    hmixT = A.alloc([128, 32, HT], BF16)
    y = A.alloc([128, NT4, D], F32)
    wbuf = [A.alloc([128, 32, 256], BF16) for _ in range(2)]
    hmb = A.alloc([128, 2048], F32)
    ogb = A.alloc([128, 2048], F32)
    xb = A.alloc([128, D], F32)
    junk = A.alloc([128, D], BF16)
    ssh = A.alloc([128, 8], F32)
    ssy = A.alloc([128, 8], F32)
    wrot = Rot(wbuf)
    prot = Rot(pb[0:4])
    prot2 = Rot(pb[4:8])
    wv = wout_d.rearrange("(kc p) n -> p kc n", p=128)
    hav = haT_d.rearrange("(c p) t -> p c t", p=128)
    tz = S.op("vector", lambda e: e.memset(ssh, 0.0))
    tz2 = S.op("vector", lambda e: e.memset(ssy, 0.0))
    prev_half = []
    hm_free = []
    x_free = []
    junk_free = [tz2]
    for half in range(TPC // HT):
        t0 = half * HT
        tha = S.dma("gpsimd", "mx_ha", hmixT[:, 16:32, :], hav[:, :, t0:t0 + HT], deps=prev_half)
        tr_toks = []
        for t4 in range(NT4):
            r0 = t0 + t4 * 128
            tl1 = S.dma("sync", "mx_hm", hmb, hm_d[r0:r0 + 128, :], deps=hm_free)
            tl2 = S.dma("sync", "mx_og", ogb, og_d[r0:r0 + 128, :], deps=hm_free)
            tsq = None
            for h in range(4):
                tsq = S.op("scalar", lambda e, h=h: e.activation(junk[:, 0:512], hmb[:, h * 512:(h + 1) * 512],
                                                                 AF.Square, accum_out=ssh[:, h:h + 1]),
                           deps=[tl1, tz] + junk_free + prev_half)
            junk_free = [tsq]
            tsg = S.op("scalar", lambda e: e.activation(ogb, ogb, AF.Sigmoid), deps=[tl2])
            tr = rstd_from_ss(S, ssh[:, 0:4], 512, [tsq])
            tm = None
            for h in range(4):
                tm = S.op("vector", lambda e, h=h: e.scalar_tensor_tensor(
                    hmb[:, h * 512:(h + 1) * 512], hmb[:, h * 512:(h + 1) * 512], ssh[:, h:h + 1],
                    gmo_b[:, h * 512:(h + 1) * 512], ALU.mult, ALU.mult), deps=[tr])
            tm = S.op("vector", lambda e: e.tensor_tensor(hmb, hmb, ogb, ALU.mult), deps=[tm, tsg])
            ev = None
            for c in range(16):
                kp, ps, pdeps = prot.get()
                ttr = S.op("tensor", lambda e, ps=ps, c=c: e.transpose(ps[:, 0:128], hmb[:, c * 128:(c + 1) * 128],
                                                                       cst["idf"]), deps=[tm] + pdeps)
                ev = S.op("scalar", lambda e, ps=ps, c=c, t4=t4: e.activation(
                    hmixT[:, c, t4 * 128:(t4 + 1) * 128], ps[:, 0:128], AF.Copy), deps=[ttr] + prev_half)
                prot.done(kp, ev)
            hm_free = [ev]
            tr_toks.append(ev)
        ycp = []
        lastmm = None
        for cg in range(D // 256):
            kw, wb_, wdeps = wrot.get()
            tw = S.dma("gpsimd", f"mx_w{kw}", wb_, wv[:, :, cg * 256:(cg + 1) * 256], deps=wdeps)
            for t4 in range(NT4):
                kp, ps, pdeps = prot2.get()
                for kc in range(32):
                    lastmm = S.op("tensor", lambda e, ps=ps, kc=kc, wb_=wb_, t4=t4: e.matmul(
                        ps[:, 0:256], hmixT[:, kc, t4 * 128:(t4 + 1) * 128], wb_[:, kc, :],
                        start=(kc == 0), stop=(kc == 31)),
                        deps=[tw, tha] + tr_toks + (pdeps if kc == 0 else []), signal=(kc == 31))
                ev = S.op("vector", lambda e, ps=ps, t4=t4, cg=cg: e.tensor_copy(
                    y[:, t4, cg * 256:(cg + 1) * 256], ps[:, 0:256]), deps=[lastmm] + x_free)
                prot2.done(kp, ev)
                ycp.append(ev)
            wrot.done(kw, lastmm)
        prev_half = [lastmm]
        x_free = []
        for t4 in range(NT4):
            r0 = t0 + t4 * 128
            tsq = S.op("scalar", lambda e, t4=t4: e.activation(junk, y[:, t4, :], AF.Square,
                                                              accum_out=ssy[:, t4:t4 + 1]),
                       deps=ycp + junk_free)
            junk_free = [tsq]
            tr = rstd_from_ss(S, ssy[:, t4:t4 + 1], D, [tsq])
            tlx = S.dma("sync", "mx_x", xb, x_d[r0:r0 + 128, :], deps=x_free)
            t1 = S.op("vector", lambda e, t4=t4: e.scalar_tensor_tensor(
                y[:, t4, :], y[:, t4, :], ssy[:, t4:t4 + 1], B1, ALU.mult, ALU.mult), deps=[tr])
            t2 = S.op("gpsimd", lambda e, t4=t4: e.tensor_tensor(xb, xb, y[:, t4, :], ALU.add), deps=[t1, tlx])
            to = S.dma("sync", "mx_o", x1_d[r0:r0 + 128, :], xb, deps=[t2])
            x_free = [to]
        tz = S.op("vector", lambda e: e.memset(ssh, 0.0), deps=[tsq])
        tz2 = S.op("vector", lambda e: e.memset(ssy, 0.0), deps=x_free)
        junk_free.append(tz2)
    A.release(m)


def router_norm(S, A, cst, pb, x1_d, acol, bcol, ab_tok, h2T, wr_d, br_d, GT):
    m = A.mark()
    wr = A.alloc([128, 32, 64], F32)
    brb = A.alloc([128, 64], F32)
    brow = A.alloc([1, 64], F32)
    t1 = S.dma("sync", "rt_w", wr, wr_d.rearrange("(kc p) n -> p kc n", p=128))
    t2 = S.dma("sync", "rt_b", brow, br_d)
    t3 = S.op("tensor", lambda e: e.matmul(pb[6][:, 0:64], cst["onesf"][0:1, 0:128], brow, start=True, stop=True),
              deps=[t2] + cst["tok"])
    t4 = S.op("vector", lambda e: e.tensor_copy(brb, pb[6][:, 0:64]), deps=[t3])
    sc = A.alloc([128, 64], F32); bi = A.alloc([128, 64], F32); m8 = A.alloc([128, 8, 8], F32)
    gs = A.alloc([128, 8], F32); g8 = A.alloc([128, 8], F32); gm = A.alloc([128, 8], F32)
    si = A.alloc([128, 64], F32); t8 = A.alloc([128, 8], F32); em = A.alloc([128, 64], F32)
    den = A.alloc([128, 1], F32); G = A.alloc([128, 64], F32)
    lg = pb[7]
    state = {"free": [t4], "gt": []}

    def hook(tt, c, sg, tok):
        mm = S.op("tensor", lambda e: e.matmul(lg[:, 0:64], sg, wr[:, c, :], start=(c == 0), stop=(c == 31)),
                  deps=[tok, t1] + (state["free"] if c == 0 else []))
        if c == 31:
            v = "vector"
            a = S.op("scalar", lambda e: e.activation(sc, lg[:, 0:64], AF.Sigmoid), deps=[mm] + state["gt"])
            b = S.op(v, lambda e: e.tensor_tensor(bi, sc, brb, ALU.add), deps=[a])
            for g in range(8):
                b = S.op(v, lambda e, g=g: e.max(m8[:, g, :], bi[:, g * 8:(g + 1) * 8]), deps=[b])
            b = S.op(v, lambda e: e.tensor_tensor(gs, m8[:, :, 0], m8[:, :, 1], ALU.add), deps=[b])
            b = S.op(v, lambda e: e.max(g8, gs), deps=[b])
            b = S.op(v, lambda e: e.tensor_scalar(gm, gs, g8[:, 3:4], None, ALU.is_ge), deps=[b])
            gme = bass.AP(gm.tensor, gm.offset, [gm.ap[0], [1, 8], [0, 8]])
            b = S.op(v, lambda e: e.scalar_tensor_tensor(si.rearrange("p (a b) -> p a b", a=8),
                                                         bi.rearrange("p (a b) -> p a b", a=8), 2.0, gme,
                                                         ALU.add, ALU.mult), deps=[b])
            b = S.op(v, lambda e: e.max(t8, si), deps=[b])
            b = S.op(v, lambda e: e.tensor_scalar(em, si, t8[:, 7:8], None, ALU.is_ge), deps=[b])
            b = S.op(v, lambda e: e.tensor_tensor(em, em, sc, ALU.mult), deps=[b])
            b = S.op(v, lambda e: e.reduce_sum(den, em, AX.X), deps=[b])
            b = S.op(v, lambda e: e.reciprocal(den, den), deps=[b])
            b = S.op(v, lambda e: e.tensor_scalar(G, em, den[:, 0:1], 2.5, ALU.mult, ALU.mult), deps=[b])
            tr = S.op("tensor", lambda e: e.transpose(pb[6][0:64, 0:128], G, cst["idf"]), deps=[b])
            cp = S.op("vector", lambda e: e.tensor_copy(GT[:, tt * 128:(tt + 1) * 128], pb[6][0:64, 0:128]),
                      deps=[tr])
            state["free"] = [a]
            state["gt"] = [cp]
        return [mm]

    prenorm(S, A, cst, pb, lambda tt: x1_d[tt * 128:(tt + 1) * 128, :], TPC // 128, acol, bcol, ab_tok, h2T,
            f32_hook=hook)
    A.release(m)


def moe_m1(S, A, cst, pb, h2T, GT, wg_d, wu_d, act_d):
    m = A.mark()
    FW = 256
    wgb = [A.alloc([128, 32, FW], BF16) for _ in range(2)]
    wub = [A.alloc([128, 32, FW], BF16) for _ in range(2)]
    wrot = Rot(list(zip(wgb, wub)))
    Rg = [A.alloc([64, TPC], F32) for _ in range(2)]
    rgrot = Rot(Rg)
    sb = [A.alloc([128, 512], F32) for _ in range(2)]
    srot = Rot(sb)
    tb = [A.alloc([128, 512], F32) for _ in range(2)]
    trot = Rot(tb)
    stg = [A.alloc([128, TPC], BF16) for _ in range(2)]
    stgrot = Rot(stg)
    gurot = Rot([(pb[0], pb[1]), (pb[2], pb[3])])
    gbrot = Rot([(pb[4], pb[5]), (pb[6], pb[7])])
    for e_ in range(NE):
        gb = None
        gbt = []
        if e_ < 64:
            kr, rg, rdeps = rgrot.get()
            t = S.op("gpsimd", lambda e, rg=rg, e_=e_: e.tensor_scalar(rg, GT, cst["idf"][0:64, e_:e_ + 1], None,
                                                                       ALU.mult), deps=rdeps)
            kg, gb, gdeps = gbrot.get()
            for th in range(2):
                t2 = S.op("tensor", lambda e, gb=gb, th=th, rg=rg: e.matmul(
                    gb[th][:, :], cst["onesf"][0:64, 0:128], rg[:, th * 512:(th + 1) * 512], start=True, stop=True),
                    deps=[t] + gdeps)
                gbt.append(t2)
            rgrot.done(kr, gbt[-1])
        gb_last = None
        for fp in range(2):
            kw, (wg_, wu_), wdeps = wrot.get()
            tw1 = S.dma("gpsimd", f"m1_g{kw}", wg_, wg_d[e_].rearrange("(kc p) n -> p kc n", p=128)[:, :, fp * FW:(fp + 1) * FW],
                        deps=wdeps)
            tw2 = S.dma("gpsimd", f"m1_u{kw}", wu_, wu_d[e_].rearrange("(kc p) n -> p kc n", p=128)[:, :, fp * FW:(fp + 1) * FW],
                        deps=wdeps)
            lastmm = None
            for f2 in range(2):
                f = fp * 2 + f2
                kst, sg, stdeps = stgrot.get()
                evs = []
                for th in range(2):
                    kp, (psg, psu), pdeps = gurot.get()
                    for which, ps, w_, tw in ((0, psg, wg_, tw1), (1, psu, wu_, tw2)):
                        for kc in range(32):
                            lastmm = S.op("tensor", lambda e, ps=ps, w_=w_, kc=kc, f2=f2, th=th: e.matmul(
                                ps[:, :], w_[:, kc, f2 * 128:(f2 + 1) * 128], h2T[:, kc, th * 512:(th + 1) * 512],
                                start=(kc == 0), stop=(kc == 31)),
                                deps=[tw] + (pdeps if kc == 0 else []), signal=(kc == 31))
                    ks, s_, sdeps = srot.get()
                    a1 = S.op("scalar", lambda e, s_=s_, psg=psg: e.activation(s_, psg[:, :], AF.Silu),
                              deps=[lastmm] + sdeps)
                    if e_ < 64:
                        kt, t_, tdeps = trot.get()
                        a2 = S.op("vector", lambda e, t_=t_, s_=s_, psu=psu: e.tensor_tensor(t_, psu[:, :], s_, ALU.mult),
                                  deps=[a1] + tdeps)
                        a3 = S.op("vector", lambda e, t_=t_, gb=gb, th=th, sg=sg: e.tensor_tensor(
                            sg[:, th * 512:(th + 1) * 512], t_, gb[th][:, :], ALU.mult), deps=[a2, gbt[th]] + stdeps)
                        trot.done(kt, a3)
                        gb_last = a3
                    else:
                        a2 = a3 = S.op("vector", lambda e, s_=s_, psu=psu, th=th, sg=sg: e.tensor_tensor(
                            sg[:, th * 512:(th + 1) * 512], psu[:, :], s_, ALU.mult), deps=[a1] + stdeps)
                    srot.done(ks, a2)
                    gurot.done(kp, a2)
                    evs.append(a3)
                row0 = (e_ * 4 + f) * 128
                to = S.dma("sync", f"m1_o{kst}", act_d[row0:row0 + 128, :], sg, deps=evs)
                stgrot.done(kst, to)
            wrot.done(kw, lastmm)
        if e_ < 64:
            gbrot.done(kg, gb_last)
    A.release(m)


def moe_m2_final(S, A, cst, pb, act_d, wd_d, x1_d, B2, out_d):
    m = A.mark()
    C = A.alloc([128, 8, D], F32)
    ab = [A.alloc([128, 8, TPC], BF16) for _ in range(2)]
    wb = [A.alloc([128, 8, 512], BF16) for _ in range(2)]
    arot = Rot(ab)
    wrot = Rot(wb)
    prot = Rot(pb)
    blocks = [(e_, min(2, NE - e_)) for e_ in range(0, NE, 2)]
    lastc = None
    for bi_, (e0, ne) in enumerate(blocks):
        nk = ne * 4
        ka, a_, adeps = arot.get()
        ta = S.dma("sync", f"m2_a{ka}", a_[:, 0:nk, :],
                   act_d[e0 * 512:(e0 + ne) * 512, :].rearrange("(k p) t -> p k t", p=128), deps=adeps)
        lastmm = None
        for dc in range(8):
            kw, w_, wdeps = wrot.get()
            tws = []
            for j in range(ne):
                tws.append(S.dma("gpsimd", f"m2_w{kw}_{j}", w_[:, j * 4:(j + 1) * 4, :],
                                 wd_d[e0 + j].rearrange("(k p) n -> p k n", p=128)[:, :, dc * 512:(dc + 1) * 512],
                                 deps=wdeps))
            for tt in range(8):
                kp, ps, pdeps = prot.get()
                for k in range(nk):
                    lastmm = S.op("tensor", lambda e, ps=ps, a_=a_, w_=w_, k=k, tt=tt: e.matmul(
                        ps[:, :], a_[:, k, tt * 128:(tt + 1) * 128], w_[:, k, :], start=(k == 0), stop=(k == nk - 1)),
                        deps=[ta] + tws + (pdeps if k == 0 else []), signal=(k == nk - 1))
                if bi_ == 0:
                    lastc = S.op("vector", lambda e, ps=ps, tt=tt, dc=dc: e.tensor_copy(
                        C[:, tt, dc * 512:(dc + 1) * 512], ps[:, :]), deps=[lastmm])
                else:
                    lastc = S.op("vector", lambda e, ps=ps, tt=tt, dc=dc: e.tensor_tensor(
                        C[:, tt, dc * 512:(dc + 1) * 512], C[:, tt, dc * 512:(dc + 1) * 512], ps[:, :], ALU.add),
                        deps=[lastmm])
                prot.done(kp, lastc)
            wrot.done(kw, lastmm)
        arot.done(ka, lastmm)
    S.fence()
    A.release(m)
    C = A.alloc([128, 8, D], F32)
    xb = [A.alloc([128, D], F32) for _ in range(2)]
    junk = A.alloc([128, D], BF16)
    ssy = A.alloc([128, 8], F32)
    xrot = Rot(xb)
    tz = S.op("vector", lambda e: e.memset(ssy, 0.0))
    jf = [tz]
    outs = []
    for tt in range(8):
        kx, xt, xdeps = xrot.get()
        tl = S.dma("sync", f"fin_x{kx}", xt, x1_d[tt * 128:(tt + 1) * 128, :], deps=xdeps)
        tsq = S.op("scalar", lambda e, tt=tt: e.activation(junk, C[:, tt, :], AF.Square, accum_out=ssy[:, tt:tt + 1]),
                   deps=jf)
        jf = [tsq]
        tr = rstd_from_ss(S, ssy[:, tt:tt + 1], D, [tsq])
        t1 = S.op("vector", lambda e, tt=tt: e.scalar_tensor_tensor(
            C[:, tt, :], C[:, tt, :], ssy[:, tt:tt + 1], B2, ALU.mult, ALU.mult), deps=[tr])
        t2 = S.op("gpsimd", lambda e, tt=tt, xt=xt: e.tensor_tensor(xt, xt, C[:, tt, :], ALU.add), deps=[t1, tl])
        to = S.dma("sync", f"fin_o{kx}", out_d[tt * 128:(tt + 1) * 128, :], xt, deps=[t2])
        xrot.done(kx, to)
        outs.append(to)
    return outs


QSCALE = 192.0 ** -0.5


def rope_tables_T():
    pos = np.arange(SEQ, dtype=np.float32)
    inv = (1.0 / (np.float32(10000.0) ** (np.arange(0, 64, 2, dtype=np.float32) / np.float32(64)))).astype(np.float32)
    ang = pos[:, None] * inv[None, :]
    cos = np.cos(ang).astype(np.float32).T
    sin = np.sin(ang).astype(np.float32).T
    cs = np.concatenate([cos, cos], 0)
    sn = np.concatenate([-sin, sin], 0)
    return np.ascontiguousarray(cs), np.ascontiguousarray(sn)


def mla_part(nc, S, A, cst, pb, ptb, io):
    (cqT_d, ckvT_d, krT_d, krsT_d, gq_d, gkv_d, wuq_d, wukv_d, cs_d, sn_d, haT_o) = io
    NB = SEQ // 512
    m0 = A.mark()
    wq = A.alloc([128, 6, 512], BF16)
    wkv = A.alloc([128, 4, 512], BF16)
    gq = A.alloc([128, 6], F32)
    gkv = A.alloc([128, 4], F32)
    mA = A.alloc([1, 128], BF16)
    mB = A.alloc([1, 128], BF16)
    kpe = A.alloc([64, SEQ], BF16)
    tw = [S.dma("gpsimd", "ml_wq", wq, wuq_d.rearrange("(c p) n -> p c n", p=128)),
          S.dma("gpsimd", "ml_wkv", wkv, wukv_d.rearrange("(c p) n -> p c n", p=128)),
          S.dma("sync", "ml_gq", gq, gq_d), S.dma("sync", "ml_gkv", gkv, gkv_d)]
    tw.append(S.op("vector", lambda e: e.memset(mA, 0.0)))
    tw.append(S.op("vector", lambda e: e.memset(mA[0:1, 0:64], 1.0), deps=[tw[-1]]))
    tw.append(S.op("vector", lambda e: e.memset(mB, 0.0)))
    tw.append(S.op("vector", lambda e: e.memset(mB[0:1, 64:128], -30000.0), deps=[tw[-1]]))
    tw += cst["tok"]
    cqv = cqT_d.rearrange("(c p) t -> p c t", p=128)
    ckvv = ckvT_d.rearrange("(c p) t -> p c t", p=128)
    outs = []
    for hd in range(2):
        mh = A.mark()
        qn = A.alloc([128, SEQ], BF16)
        qr = A.alloc([64, SEQ], BF16)
        kn = A.alloc([128, SEQ], BF16)
        v = A.alloc([128, SEQ // 128, 128], BF16)
        mp = A.mark()
        cqb = A.alloc([128, 6, 512], BF16); ckvb = A.alloc([128, 4, 512], BF16)
        sq = A.alloc([128, 6, 512], BF16)
        rq = A.alloc([128, 512], F32); rkv = A.alloc([128, 512], F32)
        krb = A.alloc([64, 512], F32); krsb = A.alloc([64, 512], F32)
        csb = A.alloc([64, 512], F32); snb = A.alloc([64, 512], F32)
        xr = A.alloc([64, 512], F32); xs = A.alloc([64, 512], F32)
        prot = Rot(pb[0:6])
        blk_free = []
        for b in range(NB):
            t0 = b * 512
            sl = slice(t0, t0 + 512)
            l1 = S.dma("gpsimd", "ml_cq", cqb, cqv[:, :, sl], deps=blk_free)
            l2 = S.dma("gpsimd", "ml_ckv", ckvb, ckvv[:, :, sl], deps=blk_free)
            l3 = S.dma("sync", "ml_cs", csb, cs_d[:, sl], deps=blk_free)
            l4 = S.dma("sync", "ml_sn", snb, sn_d[:, sl], deps=blk_free)
            fin = []
            for (lat, nch, r_, gg, nfeat, extra, ld) in ((cqb, 6, rq, gq, 768, QSCALE, l1), (ckvb, 4, rkv, gkv, 512, 1.0, l2)):
                a = S.op("scalar", lambda e, lat=lat, nch=nch: e.activation(sq[:, 0:nch, :], lat, AF.Square),
                         deps=[ld] + blk_free)
                kp, ps, pdeps = prot.get()
                mm = None
                for c in range(nch):
                    mm = S.op("tensor", lambda e, ps=ps, c=c, nch=nch: e.matmul(ps[:, :], cst["onesb"], sq[:, c, :],
                                                                                start=(c == 0), stop=(c == nch - 1)),
                              deps=[a] + pdeps + tw, signal=(c == nch - 1))
                d1 = S.op("vector", lambda e, ps=ps, r_=r_, nfeat=nfeat: e.tensor_scalar(
                    r_, ps[:, :], 1.0 / nfeat, EPS, ALU.mult, ALU.add), deps=[mm] + blk_free)
                prot.done(kp, d1)
                d2 = S.op("scalar", lambda e, r_=r_: e.activation(r_, r_, AF.Sqrt), deps=[d1])
                d3 = S.op("vector", lambda e, r_=r_: e.reciprocal(r_, r_), deps=[d2])
                if extra != 1.0:
                    d3 = S.op("vector", lambda e, r_=r_, extra=extra: e.tensor_scalar(r_, r_, extra, None, ALU.mult),
                              deps=[d3])
                for c in range(nch):
                    d3 = S.op("vector", lambda e, lat=lat, c=c, gg=gg, r_=r_: e.scalar_tensor_tensor(
                        lat[:, c, :], lat[:, c, :], gg[:, c:c + 1], r_, ALU.mult, ALU.mult), deps=[d3] + tw)
                if lat is cqb:
                    nq = d3
                else:
                    nkv = d3
            c0 = hd * 256
            kp, ps, pdeps = prot.get()
            for c in range(6):
                mm = S.op("tensor", lambda e, ps=ps, c=c: e.matmul(ps[:, :], wq[:, c, c0:c0 + 128], cqb[:, c, :],
                                                                   start=(c == 0), stop=(c == 5)),
                          deps=[nq] + pdeps, signal=(c == 5))
            ev = S.op("scalar", lambda e, ps=ps, sl=sl: e.activation(qn[:, sl], ps[:, :], AF.Copy), deps=[mm])
            prot.done(kp, ev); fin.append(ev)
            kp1, ps1, pd1 = prot.get()
            kp2, ps2, pd2 = prot.get()
            for (ps, off, pd) in ((ps1, 128, pd1), (ps2, 192, pd2)):
                for c in range(6):
                    mm = S.op("tensor", lambda e, ps=ps, c=c, off=off: e.matmul(
                        ps[0:64, :], wq[:, c, c0 + off:c0 + off + 64], cqb[:, c, :], start=(c == 0), stop=(c == 5)),
                        deps=[nq] + pd, signal=(c == 5))
            e1 = S.op("vector", lambda e, ps1=ps1: e.tensor_tensor(xr, ps1[0:64, :], csb, ALU.mult),
                      deps=[mm, l3] + blk_free)
            e2 = S.op("vector", lambda e, ps2=ps2: e.tensor_tensor(xs, ps2[0:64, :], snb, ALU.mult),
                      deps=[mm, l4] + blk_free)
            prot.done(kp1, e1); prot.done(kp2, e2)
            e3 = S.op("gpsimd", lambda e, sl=sl: e.tensor_tensor(qr[:, sl], xr, xs, ALU.add), deps=[e1, e2])
            fin.append(e3)
            kp, ps, pdeps = prot.get()
            for c in range(4):
                mm = S.op("tensor", lambda e, ps=ps, c=c: e.matmul(ps[:, :], wkv[:, c, c0:c0 + 128], ckvb[:, c, :],
                                                                   start=(c == 0), stop=(c == 3)),
                          deps=[nkv] + pdeps, signal=(c == 3))
            ev = S.op("scalar", lambda e, ps=ps, sl=sl: e.activation(kn[:, sl], ps[:, :], AF.Copy), deps=[mm])
            prot.done(kp, ev); fin.append(ev)
            kp, ps, pdeps = prot.get()
            for t4 in range(4):
                for c in range(4):
                    mm = S.op("tensor", lambda e, ps=ps, c=c, t4=t4: e.matmul(
                        ps[:, t4 * 128:(t4 + 1) * 128], ckvb[:, c, t4 * 128:(t4 + 1) * 128],
                        wkv[:, c, c0 + 128:c0 + 256], start=(c == 0), stop=(c == 3)),
                        deps=[nkv] + pdeps, signal=(c == 3 and t4 == 3))
            ev = S.op("vector", lambda e, ps=ps, b=b: e.tensor_copy(
                v[:, b * 4:(b + 1) * 4, :], ps[:, :].rearrange("p (a b) -> p a b", a=4)), deps=[mm])
            prot.done(kp, ev); fin.append(ev)
            if hd == 0:
                l5 = S.dma("sync", "ml_kr", krb, krT_d[:, sl], deps=blk_free)
                l6 = S.dma("sync", "ml_krs", krsb, krsT_d[:, sl], deps=blk_free)
                g1 = S.op("gpsimd", lambda e: e.tensor_tensor(krb, krb, csb, ALU.mult), deps=[l5, l3])
                g2 = S.op("gpsimd", lambda e: e.tensor_tensor(krsb, krsb, snb, ALU.mult), deps=[l6, l4])
                g3 = S.op("gpsimd", lambda e, sl=sl: e.tensor_tensor(kpe[:, sl], krb, krsb, ALU.add), deps=[g1, g2])
                fin.append(g3)
            blk_free = fin
        S.fence()
        A.release(mp)
        p = A.alloc([128, SEQ], F32)
        Pb = A.alloc([128, SEQ], BF16)
        PT = [A.alloc([128, 1024], BF16) for _ in range(2)]
        ostg = [A.alloc([128, 128], F32) for _ in range(2)]
        mx = A.alloc([128, 32], F32); nmx = A.alloc([128, 32], F32); l_ = A.alloc([128, 32], F32)
        f_ = A.alloc([128, 32], F32); M_ = A.alloc([128, 4], F32)
        tz = S.op("vector", lambda e: e.memset(l_, 0.0))
        srot = Rot(pb[0:4])
        orot = Rot(pb[4:6])
        ptrot = Rot(list(zip(ptb, PT)))
        ostrot = Rot(ostg)
        p_free = [tz]
        pb_free = []
        small_free = [tz]
        for qb in range(SEQ // 128):
            qs = slice(qb * 128, (qb + 1) * 128)
            chunks = [(k0, min(512, qb * 128 - k0), False) for k0 in range(0, qb * 128, 512)]
            chunks.append((qb * 128, 128, True))
            n = len(chunks)
            exps = []
            for i, (k0, w, diag) in enumerate(chunks):
                kp, ps, pdeps = srot.get()
                S.op("tensor", lambda e, ps=ps, k0=k0, w=w: e.matmul(ps[:, 0:w], qn[:, qs], kn[:, k0:k0 + w],
                                                                    start=True, stop=False),
                     deps=pdeps, signal=False)
                mm = S.op("tensor", lambda e, ps=ps, k0=k0, w=w, diag=diag: e.matmul(
                    ps[:, 0:w], qr[:, qs], kpe[:, k0:k0 + w], start=False, stop=(not diag)), signal=(not diag))
                if diag:
                    mm = S.op("tensor", lambda e, ps=ps: e.matmul(ps[:, 0:128], mA, mB, start=False, stop=True),
                              deps=tw)
                r1 = S.op("vector", lambda e, ps=ps, w=w, i=i: e.reduce_max(mx[:, i:i + 1], ps[:, 0:w], AX.X),
                          deps=[mm] + small_free)
                r2 = S.op("vector", lambda e, i=i: e.tensor_scalar(nmx[:, i:i + 1], mx[:, i:i + 1], -1.0, None, ALU.mult),
                          deps=[r1])
                ex = S.op("scalar", lambda e, ps=ps, k0=k0, w=w, i=i: e.activation(
                    p[:, k0:k0 + w], ps[:, 0:w], AF.Exp, bias=nmx[:, i:i + 1], accum_out=l_[:, i:i + 1]),
                    deps=[r2] + p_free)
                srot.done(kp, ex)
                exps.append(ex)
            a1 = S.op("vector", lambda e, n=n: e.reduce_max(M_[:, 0:1], mx[:, 0:n], AX.X), deps=exps)
            a2 = S.op("vector", lambda e: e.tensor_scalar(M_[:, 0:1], M_[:, 0:1], -1.0, None, ALU.mult), deps=[a1])
            a3 = S.op("scalar", lambda e, n=n: e.activation(f_[:, 0:n], mx[:, 0:n], AF.Exp, bias=M_[:, 0:1]),
                      deps=[a2])
            a4 = S.op("vector", lambda e, n=n: e.tensor_tensor(l_[:, 0:n], l_[:, 0:n], f_[:, 0:n], ALU.mult),
                      deps=[a3])
            a5 = S.op("vector", lambda e, n=n: e.reduce_sum(M_[:, 1:2], l_[:, 0:n], AX.X), deps=[a4])
            a6 = S.op("vector", lambda e: e.reciprocal(M_[:, 1:2], M_[:, 1:2]), deps=[a5])
            a7 = S.op("vector", lambda e, n=n: e.tensor_scalar(f_[:, 0:n], f_[:, 0:n], M_[:, 1:2], None, ALU.mult),
                      deps=[a6])
            a8 = S.op("vector", lambda e: e.memset(l_, 0.0), deps=[a7])
            nrm = []
            for i, (k0, w, diag) in enumerate(chunks):
                eng = "vector" if i % 2 == 0 else "gpsimd"
                nrm.append(S.op(eng, lambda e, k0=k0, w=w, i=i: e.tensor_scalar(
                    Pb[:, k0:k0 + w], p[:, k0:k0 + w], f_[:, i:i + 1], None, ALU.mult), deps=[a7] + pb_free))
            p_free = nrm
            small_free = nrm + [a8]
            nkb = qb + 1
            ko, ops_, odeps = orot.get()
            lastpv = None
            trs = []
            for g0 in range(0, nkb, 8):
                gn = min(8, nkb - g0)
                kt, (ptp, pts), tdeps = ptrot.get()
                tr = None
                for j in range(gn):
                    kb = g0 + j
                    tr = S.op("tensor", lambda e, ptp=ptp, j=j, kb=kb: e.transpose(
                        ptp[:, j * 128:(j + 1) * 128], Pb[:, kb * 128:(kb + 1) * 128], cst["idb"]),
                        deps=nrm + tdeps, signal=(j == gn - 1))
                trs.append(tr)
                cp = S.op("scalar", lambda e, ptp=ptp, pts=pts, gn=gn: e.activation(
                    pts[:, 0:gn * 128], ptp[:, 0:gn * 128], AF.Copy), deps=[tr])
                for j in range(gn):
                    kb = g0 + j
                    lastpv = S.op("tensor", lambda e, ops_=ops_, pts=pts, j=j, kb=kb: e.matmul(
                        ops_[:, 0:128], v[:, kb, :], pts[:, j * 128:(j + 1) * 128],
                        start=(kb == 0), stop=(kb == nkb - 1)),
                        deps=[cp] + (odeps if kb == 0 else []), signal=(j == gn - 1))
                ptrot.done(kt, lastpv)
            pb_free = [trs[-1]]
            ks, og_, sdeps = ostrot.get()
            ev = S.op("vector", lambda e, og_=og_, ops_=ops_: e.tensor_copy(og_, ops_[:, 0:128]),
                      deps=[lastpv] + sdeps)
            orot.done(ko, ev)
            to = S.dma("sync", f"ml_o{ks}", haT_o[hd * 128:(hd + 1) * 128, qs], og_, deps=[ev])
            ostrot.done(ks, to)
            outs.append(to)
        S.fence()
        A.release(mh)
    A.release(m0)
    return outs


def mlstm_part(nc, S, A, cst, pb, ptb, io):
    (qT_d, kT_d, cw_d, cb_d, v_d, gates_d, bg_d, hm_o) = io
    NCH = SEQ // 64
    m0 = A.mark()
    qT = A.alloc([128, 2, SEQ], BF16); kT = A.alloc([128, 2, SEQ], BF16)
    qdT = A.alloc([128, 2, SEQ], BF16)
    DmT = A.alloc([64, NCH, 64], BF16)
    aT = A.alloc([64, 128], F32); emtT = A.alloc([64, 128], F32); wT = A.alloc([64, 128], F32)
    cdb = A.alloc([128, 128], F32)
    cw = A.alloc([128, 4, 4], F32); cb = A.alloc([128, 4], F32)
    tcw = [S.dma("sync", "ms_cw", cw, cw_d), S.dma("sync", "ms_cb", cb, cb_d)] + cst["tok"]
    mc = A.mark()
    SG = 2048
    raw = A.alloc([128, 4, SG + 4], F32)
    acc = A.alloc([128, 4, SG], F32)
    srcs = [qT_d[0:128], qT_d[128:256], kT_d[0:128], kT_d[128:256]]
    seg_free = []
    for sg_ in range(SEQ // SG):
        t0 = sg_ * SG
        fin = []
        for ch in range(4):
            eng = "vector"
            ld = [S.dma("sync", f"ms_r{ch}", raw[:, ch, 4:4 + SG], srcs[ch][:, t0:t0 + SG], deps=seg_free)]
            if sg_ == 0:
                ld.append(S.op(eng, lambda e, ch=ch: e.memset(raw[:, ch, 0:4], 0.0), deps=seg_free))
            else:
                ld.append(S.dma("sync", f"ms_h{ch}", raw[:, ch, 1:4], srcs[ch][:, t0 - 3:t0], deps=seg_free))
            a = S.op(eng, lambda e, ch=ch: e.tensor_scalar(acc[:, ch, :], raw[:, ch, 4:4 + SG], cw[:, ch, 3:4],
                                                           cb[:, ch:ch + 1], ALU.mult, ALU.add), deps=ld + tcw + seg_free)
            for j in (2, 1, 0):
                a = S.op(eng, lambda e, ch=ch, j=j: e.scalar_tensor_tensor(
                    acc[:, ch, :], raw[:, ch, 1 + j:1 + j + SG], cw[:, ch, j:j + 1], acc[:, ch, :], ALU.mult, ALU.add),
                    deps=[a])
            if ch < 2:
                s1 = S.op("scalar", lambda e, ch=ch: e.activation(acc[:, ch, :], acc[:, ch, :], AF.Silu), deps=[a])
                s2 = S.op(eng, lambda e, ch=ch, t0=t0: e.tensor_scalar(qT[:, ch, t0:t0 + SG], acc[:, ch, :], 0.0625, None,
                                                                       ALU.mult), deps=[s1])
            else:
                s2 = S.op("scalar", lambda e, ch=ch, t0=t0: e.activation(kT[:, ch - 2, t0:t0 + SG], acc[:, ch, :], AF.Silu),
                          deps=[a])
            fin.append(s2)
        seg_free = fin
    S.fence()
    A.release(mc)
    mg = A.mark()
    al = lambda: A.alloc([128, 64], F32)
    gi, gf, li, lf, b_, a_, cm, M_, t_, dec, w_ = [al() for _ in range(11)]
    ones64 = al()
    bg = A.alloc([128, 2], F32); col = A.alloc([128, 8], F32)
    rows = A.alloc([1, 4, 128], F32)
    R = A.alloc([128, NCH, 64], F32)
    mk2 = A.alloc([64, 512], F32)
    mk_d = nc.inline_tensor(np.tile(np.tril(np.full((64, 64), 30000.0, np.float32), -1), (1, 8)), "c_mask")
    V = "vector"
    l0 = [S.dma("sync", "mg_i", gi, gates_d[0]), S.dma("sync", "mg_f", gf, gates_d[1]),
          S.dma("sync", "mg_b", bg, bg_d), S.dma("sync", "mg_m", mk2, mk_d.ap())]
    x = S.op(V, lambda e: e.memset(ones64, 1.0))
    x = S.op(V, lambda e: e.tensor_scalar(bg, bg, 1.0 / 15.0, None, ALU.mult), deps=l0 + [x])
    y1 = S.op("scalar", lambda e: e.activation(li, gi, AF.Tanh, bias=bg[:, 0:1], scale=1.0 / 15.0), deps=[x])
    y2 = S.op("scalar", lambda e: e.activation(lf, gf, AF.Tanh, bias=bg[:, 1:2], scale=1.0 / 15.0), deps=[x])
    x = S.op(V, lambda e: e.tensor_scalar(li, li, 15.0, None, ALU.mult), deps=[y1])
    y = S.op("scalar", lambda e: e.activation(lf, lf, AF.Exp, scale=-15.0), deps=[y2])
    x = S.op(V, lambda e: e.tensor_scalar(lf, lf, 1.0, None, ALU.add), deps=[y, x])
    y = S.op("scalar", lambda e: e.activation(lf, lf, AF.Ln), deps=[x])
    x = S.op(V, lambda e: e.tensor_scalar(lf, lf, -1.0, None, ALU.mult), deps=[y])
    x = S.op(V, lambda e: e.tensor_tensor_scan(b_, ones64, lf, 0.0, ALU.mult, ALU.add), deps=[x])
    x = S.op(V, lambda e: e.tensor_tensor(a_, li, b_, ALU.subtract), deps=[x])
    x = S.op(V, lambda e: e.tensor_tensor_scan(cm, a_, a_, -1e30, ALU.max, ALU.max), deps=[x])
    pr = pb[0]
    mm = S.op("tensor", lambda e: e.matmul(pr[0:1, 0:128], cm[:, 63:64], cst["idf"], start=True, stop=True), deps=[x])
    mm = S.op("tensor", lambda e: e.matmul(pr[0:1, 128:256], b_[:, 63:64], cst["idf"], start=True, stop=True), deps=[mm])
    x = S.op(V, lambda e: e.tensor_copy(rows[0:1, 0:2, :], pr[0:1, 0:256].rearrange("p (a b) -> p a b", a=2)), deps=[mm])
    x = S.op(V, lambda e: e.tensor_tensor_scan(rows[0:1, 2, :], rows[0:1, 0, :], rows[0:1, 1, :], 0.0, ALU.max, ALU.add),
             deps=[x])
    x0 = S.op(V, lambda e: e.memset(rows[0:1, 3, 0:1], 0.0), deps=[x])
    x = S.op(V, lambda e: e.tensor_copy(rows[0:1, 3, 1:128], rows[0:1, 2, 0:127]), deps=[x0])
    mm = S.op("tensor", lambda e: e.matmul(pr[:, 256:257], rows[0:1, 3, :], cst["onesf"][0:1, 0:1], start=True, stop=True),
              deps=[x])
    mm = S.op("tensor", lambda e: e.matmul(pr[:, 257:258], rows[0:1, 2, :], cst["onesf"][0:1, 0:1], start=True, stop=True),
              deps=[mm])
    x = S.op(V, lambda e: e.tensor_copy(col[:, 0:2], pr[:, 256:258]), deps=[mm])
    x = S.op(V, lambda e: e.tensor_scalar(M_, cm, col[:, 0:1], None, ALU.max), deps=[x])
    x = S.op(V, lambda e: e.tensor_tensor(t_, b_, M_, ALU.add), deps=[x])
    y = S.op("scalar", lambda e: e.activation(t_, t_, AF.Exp, scale=-1.0), deps=[x])
    y = S.op("scalar", lambda e: e.activation(dec, M_, AF.Exp, scale=-1.0, bias=col[:, 0:1]), deps=[y])
    x = S.op(V, lambda e: e.tensor_tensor(col[:, 2:3], b_[:, 63:64], col[:, 1:2], ALU.subtract), deps=[y])
    y = S.op("scalar", lambda e: e.activation(w_, a_, AF.Exp, bias=col[:, 2:3]), deps=[x])
    x = S.op(V, lambda e: e.tensor_tensor(col[:, 3:4], col[:, 2:3], col[:, 0:1], ALU.add), deps=[y])
    y = S.op("scalar", lambda e: e.activation(col[:, 4:5], col[:, 3:4], AF.Exp), deps=[x])
    for src, dst, k in ((a_, aT, 0), (t_, emtT, 1), (w_, wT, 2)):
        mm = S.op("tensor", lambda e, src=src, k=k: e.transpose(pb[1 + k][0:64, 0:128], src, cst["idf"]),
                  deps=[y])
        x = S.op(V, lambda e, dst=dst, k=k: e.tensor_copy(dst, pb[1 + k][0:64, 0:128]), deps=[mm])
    x = S.op(V, lambda e: e.tensor_scalar(R[:, 0, :], cst["idf"][:, 0:64], col[:, 4:5], None, ALU.mult), deps=[x])
    x = S.op(V, lambda e: e.tensor_scalar(R[:, 1, :], cst["idf"][:, 64:128], col[:, 4:5], None, ALU.mult), deps=[x])
    mm = S.op("tensor", lambda e: e.matmul(pb[1][:, 384:512], cst["onesf"], R[:, 0:2, :].rearrange("p a b -> p (a b)"),
                                           start=True, stop=True), deps=[x])
    x = S.op(V, lambda e: e.tensor_copy(cdb, pb[1][:, 384:512]), deps=[mm])
    idb3 = bass.AP(cst["idf"].tensor, cst["idf"].offset, [cst["idf"].ap[0], [1, 128], [0, 64]])
    Mb3 = bass.AP(M_.tensor, M_.offset, [M_.ap[0], [0, 128], [1, 64]])
    x = S.op(V, lambda e: e.tensor_tensor(R, Mb3, idb3, ALU.mult), deps=[x])
    prot = Rot(pb[2:6])
    ex = None
    for g in range(NCH // 8):
        kp, ps, pdeps = prot.get()
        S.op("tensor", lambda e, ps=ps, g=g: e.matmul(ps[0:64, :], cst["onesf"][:, 0:64],
                                                      R[:, g * 8:(g + 1) * 8, :].rearrange("p a b -> p (a b)"),
                                                      start=True, stop=False), deps=[x] + pdeps, signal=False)
        mm = S.op("tensor", lambda e, ps=ps: e.matmul(ps[0:64, :], cst["idf"][0:64, 0:64], mk2, start=False, stop=True),
                  deps=[l0[3]])
        for j in range(8):
            c = g * 8 + j
            ex = S.op("scalar", lambda e, ps=ps, j=j, c=c: e.activation(DmT[:, c, :], ps[0:64, j * 64:(j + 1) * 64], AF.Exp,
                                                                        scale=-1.0, bias=aT[:, c:c + 1]), deps=[mm])
        prot.done(kp, ex)
    xm = ex
    dc3 = bass.AP(dec.tensor, dec.offset, [dec.ap[0], [0, 128], [1, 64]])
    x = S.op(V, lambda e: e.tensor_tensor(R, dc3, idb3, ALU.mult), deps=[ex])
    lastq = None
    for g in range(NCH // 8):
        kp, ps, pdeps = prot.get()
        mm = S.op("tensor", lambda e, ps=ps, g=g: e.matmul(ps[:, :], cst["onesf"],
                                                           R[:, g * 8:(g + 1) * 8, :].rearrange("p a b -> p (a b)"),
                                                           start=True, stop=True), deps=[x] + pdeps)
        for dc in range(2):
            lastq = S.op(V, lambda e, ps=ps, g=g, dc=dc: e.tensor_tensor(
                qdT[:, dc, g * 512:(g + 1) * 512], qT[:, dc, g * 512:(g + 1) * 512], ps[:, :], ALU.mult), deps=[mm])
        prot.done(kp, lastq)
    S.fence()
    A.release(mg)
    VG = 16
    vaug = [A.alloc([64, VG, 272], BF16) for _ in range(2)]
    Cst = A.alloc([128, 2, 272], F32); Cb = A.alloc([128, 2, 272], BF16)
    SgT = [A.alloc([64, 64], BF16) for _ in range(2)]
    kw = [A.alloc([64, 256], BF16) for _ in range(2)]
    rr = A.alloc([64, 4], F32)
    hst = [A.alloc([64, 8, 256], F32) for _ in range(2)]
    i0 = [S.op(V, lambda e: e.memset(Cst, 0.0)), S.op(V, lambda e: e.memset(Cb, 0.0))]
    for vb in vaug:
        i0.append(S.op("gpsimd", lambda e, vb=vb: e.memset(vb[:, :, 256:257], 1.0)))
    vrot = Rot(vaug); sgrot = Rot(SgT); kwrot = Rot(kw); hrot = Rot(hst)
    strot = Rot(pb[0:2]); nrot = Rot(pb[2:4]); kprot = Rot(ptb)
    U = [pb[4], pb[5]]
    cb_tok = i0[1]
    c_tok = i0[0]
    u_free = []
    outs = []
    vcur = None
    hcur = None
    for c in range(NCH):
        cs = slice(c * 64, (c + 1) * 64)
        if c % VG == 0:
            kv, vb, vdeps = vrot.get()
            tv = S.dma("gpsimd", f"mr_v{kv}", vb[:, :, 0:256],
                       v_d[c * 64:(c + VG) * 64, :].rearrange("(c s) e -> s c e", s=64), deps=vdeps + i0)
            vcur = (kv, vb, tv)
        kv, vb, tv = vcur
        cg = c % VG
        if c % 8 == 0:
            kh, hb, hdeps = hrot.get()
            hcur = (kh, hb, hdeps)
        kh, hb, hdeps = hcur
        kp, stp, pdeps = strot.get()
        S.op("tensor", lambda e, stp=stp: e.matmul(stp[0:64, 0:64], kT[:, 0, cs], qT[:, 0, cs], start=True, stop=False),
             deps=pdeps, signal=False)
        mm = S.op("tensor", lambda e, stp=stp: e.matmul(stp[0:64, 0:64], kT[:, 1, cs], qT[:, 1, cs], start=False, stop=True))
        ksg, sg, sgdeps = sgrot.get()
        b1 = S.op(V, lambda e, stp=stp, sg=sg, c=c: e.tensor_tensor(sg, stp[0:64, 0:64], DmT[:, c, :], ALU.mult),
                  deps=[mm, xm] + sgdeps)
        strot.done(kp, b1)
        kk, kps, kdeps = kprot.get()
        S.op("tensor", lambda e, kps=kps: e.transpose(kps[0:64, 0:128], kT[:, 0, cs], cst["idb"]), deps=kdeps, signal=False)
        mm2 = S.op("tensor", lambda e, kps=kps: e.transpose(kps[0:64, 128:256], kT[:, 1, cs], cst["idb"]))
        kkw, kwb, kwdeps = kwrot.get()
        c1 = S.op("scalar", lambda e, kps=kps, kwb=kwb, c=c: e.activation(kwb, kps[0:64, 0:256], AF.Copy, scale=wT[:, c:c + 1]),
                  deps=[mm2] + kwdeps)
        kprot.done(kk, c1)
        kn_, nps, ndeps = nrot.get()
        S.op("tensor", lambda e, nps=nps, sg=sg, vb=vb, cg=cg: e.matmul(nps[0:64, 0:257], sg, vb[:, cg, 0:257], start=True, stop=False),
             deps=[b1, tv] + ndeps, signal=False)
        S.op("tensor", lambda e, nps=nps: e.matmul(nps[0:64, 0:257], qdT[:, 0, cs], Cb[:, 0, 0:257], start=False, stop=False),
             deps=[cb_tok], signal=False)
        mm3 = S.op("tensor", lambda e, nps=nps: e.matmul(nps[0:64, 0:257], qdT[:, 1, cs], Cb[:, 1, 0:257], start=False, stop=True))
        sgrot.done(ksg, mm3)
        e0 = S.op(V, lambda e, nps=nps: e.tensor_scalar(rr[:, 1:2], nps[0:64, 256:257], -1.0, None, ALU.mult), deps=[mm3])
        e0 = S.op(V, lambda e, nps=nps: e.tensor_tensor(rr[:, 0:1], nps[0:64, 256:257], rr[:, 1:2], ALU.max), deps=[e0])
        e1 = S.op(V, lambda e, c=c: e.tensor_scalar(rr[:, 0:1], rr[:, 0:1], emtT[:, c:c + 1], None, ALU.max), deps=[e0])
        e2 = S.op(V, lambda e: e.reciprocal(rr[:, 0:1], rr[:, 0:1]), deps=[e1])
        e3 = S.op(V, lambda e, nps=nps, hb=hb, c=c: e.tensor_scalar(hb[:, c % 8, :], nps[0:64, 0:256], rr[:, 0:1], None, ALU.mult),
                  deps=[e2] + hdeps)
        nrot.done(kn_, e3)
        if c % 8 == 7:
            to = S.dma("sync", f"mr_o{kh}", hm_o[(c - 7) * 64:(c + 1) * 64, :].rearrange("(c s) e -> s c e", s=64), hb,
                       deps=[e3])
            hrot.done(kh, to)
            outs.append(to)
        mu = None
        for dc in range(2):
            mu = S.op("tensor", lambda e, dc=dc, kwb=kwb, vb=vb, cg=cg: e.matmul(
                U[dc][:, 0:257], kwb[:, dc * 128:(dc + 1) * 128], vb[:, cg, 0:257], start=True, stop=True),
                deps=[c1, tv] + u_free, signal=(dc == 1))
        kwrot.done(kkw, mu)
        if cg == VG - 1:
            vrot.done(kv, mu)
        g_ = None
        for dc in range(2):
            g_ = S.op(V, lambda e, dc=dc, c=c: e.scalar_tensor_tensor(
                Cst[:, dc, 0:257], Cst[:, dc, 0:257], cdb[:, c:c + 1], U[dc][:, 0:257], ALU.mult, ALU.add), deps=[mu, c_tok])
        u_free = [g_]
        c_tok = g_
        cb_tok = S.op("scalar", lambda e: e.activation(Cb, Cst, AF.Copy), deps=[g_, mm3])
    A.release(m0)
    return outs


def build_phase_b(parts=("mlstm", "mla")):
    nc = bass.Bass("TRN2", target_bir_lowering=False)
    dt = lambda n, s, k="ExternalInput", d=F32: nc.dram_tensor(n, s, d, kind=k).ap()
    qT_d = dt("qT", [256, SEQ]); kT_d = dt("kT", [256, SEQ])
    cw_d = dt("cw", [128, 4, 4]); cb_d = dt("cb", [128, 4])
    v_d = dt("v", [SEQ, 256]); gates_d = dt("gates", [2, 128, 64]); bg_d = dt("bg", [128, 2])
    cqT_d = dt("cqT", [768, SEQ]); ckvT_d = dt("ckvT", [512, SEQ])
    krT_d = dt("krT", [64, SEQ]); krsT_d = dt("krsT", [64, SEQ])
    gq_d = dt("gq", [128, 6]); gkv_d = dt("gkv", [128, 4])
    wuq_d = dt("wuq", [768, 512]); wukv_d = dt("wukv", [512, 512])
    cs_d = dt("cs", [64, SEQ]); sn_d = dt("sn", [64, SEQ])
    hm_o = dt("hm", [SEQ, 256], "ExternalOutput")
    haT_o = dt("haT", [256, SEQ], "ExternalOutput")
    S = Sched(nc)
    A = Arena(nc, 196 * 1024)
    pb = psum_banks(nc, 6)
    ptb = [nc.alloc_psum_tensor(f"ptb{i}", [128, 1024], BF16) for i in range(2)]
    cst = load_consts(nc, S, A)
    o1 = o2 = []
    if "mlstm" in parts:
        o1 = mlstm_part(nc, S, A, cst, pb, ptb, (qT_d, kT_d, cw_d, cb_d, v_d, gates_d, bg_d, hm_o))
        S.fence()
    if "mla" in parts:
        o2 = mla_part(nc, S, A, cst, pb, ptb, (cqT_d, ckvT_d, krT_d, krsT_d, gq_d, gkv_d, wuq_d, wukv_d, cs_d, sn_d, haT_o))
    S.emit(o1 + o2)
    return nc


_CACHE = {}


def _prog(name, fn):
    if name not in _CACHE:
        _CACHE[name] = fn()
    return _CACHE[name]


def kernel(x, c, w_ada, b_ada, g_pre_mix, g_post_mix, w_in, conv_w, conv_b, b_igate, b_fgate,
           g_mlstm_out, g_q_norm, w_uq, g_kv_norm, w_ukv, w_out, g_pre_ffn, g_post_ffn,
           w_router, b_router, w_gate, w_up, w_down, w_shared_gate, w_shared_up, w_shared_down):
    f = lambda a: np.ascontiguousarray(np.asarray(a, dtype=np.float32))
    x = f(x); c = f(c); w_ada = f(w_ada)[0]; b_ada = f(b_ada)
    cores = list(range(NCORES))
    win_p = f(f(w_in)[0][:, win_col_perm()])
    wada_a = f(w_ada[:, 0:2 * D]); bada_a = f(b_ada[:, 0:2 * D])
    gpre = f(g_pre_mix)
    ims = [{"x": f(x[0, i * TPC:(i + 1) * TPC]), "c": c, "wada": wada_a, "bada": bada_a, "gpre": gpre, "win": win_p}
           for i in cores]
    ra = run_bass_kernel_spmd(_prog("a", build_phase_a), ims, core_ids=cores).results
    featT = np.concatenate([r["featT"] for r in ra], axis=1)
    tokm = np.concatenate([r["tokm"] for r in ra], axis=0)
    del ims, ra
    cw_all = f(conv_w)[0]; cb_all = f(conv_b)[0]
    cs, sn = rope_tables_T()
    wuq = f(w_uq)[0].reshape(768, 16, 192); wukv = f(w_ukv)[0].reshape(512, 16, 256)
    gq = f(f(g_q_norm)[0].reshape(6, 128).T); gkv = f(f(g_kv_norm)[0].reshape(4, 128).T)
    ims = []
    for j in cores:
        h, dh = j // 2, j % 2
        chans = [np.arange(h * 256, h * 256 + 128), np.arange(h * 256 + 128, h * 256 + 256),
                 np.arange(1024 + h * 256, 1024 + h * 256 + 128), np.arange(1024 + h * 256 + 128, 1024 + h * 256 + 256)]
        cw = f(np.stack([cw_all[:, ch].T for ch in chans], axis=1))
        cb = f(np.stack([cb_all[ch] for ch in chans], axis=1))
        gates = f(np.stack([tokm[:, 4096 + h].reshape(128, 64), tokm[:, 4100 + h].reshape(128, 64)], 0))
        bg = f(np.tile(np.array([[f(b_igate)[0, h], f(b_fgate)[0, h]]], np.float32), (128, 1)))
        wq_parts, wkv_parts = [], []
        for hd in range(2):
            hg = 2 * j + hd
            wq_parts += [wuq[:, hg, 0:128], wuq[:, hg, 128:192], wuq[:, hg, 160:192], wuq[:, hg, 128:160]]
            wkv_parts += [wukv[:, hg, 0:128], wukv[:, hg, 128:256]]
        ims.append({"qT": f(featT[h * 256:(h + 1) * 256]), "kT": f(featT[1024 + h * 256:1024 + (h + 1) * 256]),
                    "cw": cw, "cb": cb, "v": f(tokm[:, h * 512 + dh * 256:h * 512 + (dh + 1) * 256]),
                    "gates": gates, "bg": bg,
                    "cqT": f(featT[2048:2816]), "ckvT": f(featT[2816:3328]),
                    "krT": f(featT[3328:3392]), "krsT": f(featT[3392:3456]),
                    "gq": gq, "gkv": gkv, "wuq": f(np.concatenate(wq_parts, 1)), "wukv": f(np.concatenate(wkv_parts, 1)),
                    "cs": cs, "sn": sn})
    rb = run_bass_kernel_spmd(_prog("b", build_phase_b), ims, core_ids=cores).results
    hm_all = np.concatenate([r["hm"] for r in rb], axis=1)
    haT_all = np.concatenate([r["haT"] for r in rb], axis=0)
    del ims, rb
    wg = f(np.concatenate([f(w_gate)[0], f(w_shared_gate)], 0))
    wu = f(np.concatenate([f(w_up)[0], f(w_shared_up)], 0))
    wd = f(np.concatenate([f(w_down)[0], f(w_shared_down)], 0))
    wada_c = f(w_ada[:, 2 * D:]); bada_c = f(b_ada[:, 2 * D:])
    shared = {"c": c, "wada": wada_c, "bada": bada_c, "gpostmix": f(g_post_mix), "gpreffn": f(g_pre_ffn),
              "gpostffn": f(g_post_ffn), "gmo": f(g_mlstm_out), "wout": f(w_out)[0], "wr": f(w_router)[0],
              "br": f(b_router), "wg": wg, "wu": wu, "wd": wd}
    ims = []
    for i in cores:
        sl = slice(i * TPC, (i + 1) * TPC)
        d = dict(shared)
        d.update({"x": f(x[0, sl]), "hm": f(hm_all[sl]), "og": f(tokm[sl, 2048:4096]), "haT": f(haT_all[:, sl])})
        ims.append(d)
    rc = run_bass_kernel_spmd(_prog("c", build_phase_c), ims, core_ids=cores).results
    out = np.concatenate([r["out"] for r in rc], axis=0)[None]
    return out.astype(np.float32)
```
